# Optimizing a Trainium2 kernel written in Bass

```python
import math
import jax
import jax.numpy as jnp
from jax import lax
import numpy as np

D_MODEL = 1024
BATCH = 4
SEQ = 4096
DEPTH = 4

MOBA_HEADS = 8
MOBA_HEAD_DIM = 64
MOBA_BLOCK = 256
MOBA_TOPK = 3
MOBA_Q_CHUNK = 64
RG_WIDTH = D_MODEL
RG_BLOCKS = 16
RG_BLOCK_DIM = RG_WIDTH // RG_BLOCKS
RG_CONV = 4
RG_C = 8.0
RG_A_MIN = 0.9
RG_A_MAX = 0.999
DIFF_HEADS = 4
DIFF_HEAD_DIM = 64
DIFF_Q_BLOCK = 128
D_FF = 2816
FFN_CONV = 3
N_BRANCHES = 3
RMS_EPS = 1e-6

MOBA_WIDTH = MOBA_HEADS * MOBA_HEAD_DIM
DIFF_QK_WIDTH = DIFF_HEADS * 2 * DIFF_HEAD_DIM
DIFF_V_WIDTH = DIFF_HEADS * 2 * DIFF_HEAD_DIM
IN_WIDTHS = (MOBA_WIDTH, MOBA_WIDTH, MOBA_WIDTH, DIFF_QK_WIDTH, DIFF_QK_WIDTH, DIFF_V_WIDTH, RG_WIDTH, RG_WIDTH, N_BRANCHES * D_MODEL)
IN_COLS = sum(IN_WIDTHS)

kernel_name = 'hybrid_moba_rglru_diffattn_convffn'


def rms_norm(x, g):
    xf = x.astype(jnp.float32)
    y = xf * lax.rsqrt(jnp.mean(xf * xf, axis=-1, keepdims=True) + RMS_EPS)
    return (y * g.astype(jnp.float32)).astype(x.dtype)


def alibi_slopes(n):
    return 2.0 ** (-8.0 * jnp.arange(1, n + 1, dtype=jnp.float32) / n)


def causal_dwconv(x, w, b):
    k_w = w.shape[0]
    s = x.shape[1]
    xp = jnp.pad(x, ((0, 0), (k_w - 1, 0), (0, 0)))
    y = b
    for j in range(k_w):
        y = y + w[j] * xp[:, j:j + s]
    return y


def split_offsets():
    return np.cumsum(np.array(IN_WIDTHS[:-1])).tolist()


def moba_attention(q, k, v):
    B, S, H, D = q.shape
    nb = -(-S // MOBA_BLOCK)
    s_pad = nb * MOBA_BLOCK
    pad = ((0, 0), (0, s_pad - S), (0, 0), (0, 0))
    q, k, v = [jnp.pad(t, pad).transpose(0, 2, 1, 3) for t in (q, k, v)]
    kb = k.reshape(B, H, nb, MOBA_BLOCK, D)
    vb = v.reshape(B, H, nb, MOBA_BLOCK, D)
    slopes = alibi_slopes(H)[None, :, None, None]
    scale = D ** -0.5
    kmean = jnp.mean(kb.astype(jnp.float32), axis=3)
    gate = jnp.einsum('bhsd,bhnd->bhsn', q.astype(jnp.float32), kmean)
    q_blk = jnp.arange(s_pad) // MOBA_BLOCK
    past = jnp.arange(nb)[None, :] < q_blk[:, None]
    gate = jnp.where(past, gate, -jnp.inf)
    n_sel = min(MOBA_TOPK, nb)
    _, sel = lax.top_k(gate, n_sel)
    bi = jnp.arange(B)[:, None, None, None]
    hi = jnp.arange(H)[None, :, None, None]
    n_chunks = s_pad // MOBA_Q_CHUNK

    def chunk(c):
        q0 = c * MOBA_Q_CHUNK
        blk = q0 // MOBA_BLOCK
        qc = lax.dynamic_slice_in_dim(q, q0, MOBA_Q_CHUNK, axis=2)
        selc = lax.dynamic_slice_in_dim(sel, q0, MOBA_Q_CHUNK, axis=2)
        qpos = q0 + jnp.arange(MOBA_Q_CHUNK)
        k_own = lax.dynamic_index_in_dim(kb, blk, axis=2, keepdims=False)
        v_own = lax.dynamic_index_in_dim(vb, blk, axis=2, keepdims=False)
        kpos_own = blk * MOBA_BLOCK + jnp.arange(MOBA_BLOCK)
        dist_own = (qpos[:, None] - kpos_own[None, :]).astype(jnp.float32)
        l_own = jnp.einsum('bhqd,bhkd->bhqk', qc, k_own).astype(jnp.float32) * scale - slopes * dist_own
        l_own = jnp.where(dist_own >= 0, l_own, -jnp.inf)
        k_sel = kb[bi, hi, selc]
        v_sel = vb[bi, hi, selc]
        kpos_sel = selc[..., None] * MOBA_BLOCK + jnp.arange(MOBA_BLOCK)
        dist_sel = (qpos[None, None, :, None, None] - kpos_sel).astype(jnp.float32)
        l_sel = jnp.einsum('bhqd,bhqnkd->bhqnk', qc, k_sel).astype(jnp.float32) * scale - slopes[..., None] * dist_sel
        valid = (selc < blk)[..., None]
        l_sel = jnp.where(valid, l_sel, -jnp.inf)
        n_keys = n_sel * MOBA_BLOCK
        logits = jnp.concatenate([l_sel.reshape(B, H, MOBA_Q_CHUNK, n_keys), l_own], axis=-1)
        p = jax.nn.softmax(logits, axis=-1).astype(v.dtype)
        p_sel = p[..., :n_keys].reshape(B, H, MOBA_Q_CHUNK, n_sel, MOBA_BLOCK)
        p_own = p[..., n_keys:]
        return (jnp.einsum('bhqnk,bhqnkd->bhqd', p_sel, v_sel)
                + jnp.einsum('bhqk,bhkd->bhqd', p_own, v_own))

    out = lax.map(chunk, jnp.arange(n_chunks))
    out = out.transpose(1, 0, 3, 2, 4).reshape(B, s_pad, H, D)
    return out[:, :S]


def diff_attention(q, k, v, lam, lam_init, subln_g):
    B, S, H, _, Dh = q.shape
    q = q.transpose(0, 2, 3, 1, 4)
    k = k.transpose(0, 2, 3, 1, 4)
    v = v.transpose(0, 2, 1, 3)
    slopes = alibi_slopes(H)[None, :, None, None, None]
    scale = Dh ** -0.5
    kpos = jnp.arange(S)
    n_blocks = S // DIFF_Q_BLOCK

    def block(c):
        q0 = c * DIFF_Q_BLOCK
        qc = lax.dynamic_slice_in_dim(q, q0, DIFF_Q_BLOCK, axis=3)
        qpos = q0 + jnp.arange(DIFF_Q_BLOCK)
        dist = (qpos[:, None] - kpos[None, :]).astype(jnp.float32)
        logits = jnp.einsum('bhmqd,bhmkd->bhmqk', qc, k).astype(jnp.float32) * scale - slopes * dist
        logits = jnp.where(dist >= 0, logits, -jnp.inf)
        p = jax.nn.softmax(logits, axis=-1)
        w = p[:, :, 0] - lam * p[:, :, 1]
        return jnp.einsum('bhqk,bhkd->bhqd', w.astype(v.dtype), v)

    out = lax.map(block, jnp.arange(n_blocks))
    out = out.transpose(1, 0, 3, 2, 4).reshape(B, S, H, 2 * Dh)
    out = rms_norm(out, subln_g) * (1.0 - lam_init)
    return out.reshape(B, S, H * 2 * Dh)


def rg_lru(x, w_a, b_a, w_x, b_x, lam):
    B, S, C = x.shape
    xb = x.reshape(B, S, RG_BLOCKS, RG_BLOCK_DIM)
    r = jax.nn.sigmoid(jnp.einsum('bsnc,ncd->bsnd', xb, w_a).reshape(B, S, C) + b_a)
    i = jax.nn.sigmoid(jnp.einsum('bsnc,ncd->bsnd', xb, w_x).reshape(B, S, C) + b_x)
    log_a = -RG_C * r.astype(jnp.float32) * jax.nn.softplus(-lam.astype(jnp.float32))
    a = jnp.exp(log_a)
    mult = jnp.sqrt(-jnp.expm1(2.0 * log_a))
    b = mult * (i * x).astype(jnp.float32)

    def combine(left, right):
        a1, b1 = left
        a2, b2 = right
        return a1 * a2, a2 * b1 + b2

    _, h = lax.associative_scan(combine, (a, b), axis=1)
    return h.astype(x.dtype)


def setup_inputs(seed: int = 0) -> dict:
    key = jax.random.key(seed)
    ks = jax.random.split(key, 26)
    f32 = jnp.float32
    L = DEPTH

    def nrm(k, shape, scale):
        return jax.random.normal(k, shape, f32) * scale

    def gain(k, shape):
        return 1.0 + nrm(k, shape, 0.01)

    a_c = jax.random.uniform(ks[10], (L, RG_WIDTH), f32, RG_A_MIN, RG_A_MAX)
    a0 = a_c ** (1.0 / RG_C)
    return {
        'x': nrm(ks[0], (BATCH, SEQ, D_MODEL), 1.0),
        'norm_mix_g': gain(ks[1], (L, D_MODEL)),
        'w_in': nrm(ks[2], (L, D_MODEL, IN_COLS), D_MODEL ** -0.5),
        'b_gate': nrm(ks[3], (L, N_BRANCHES * D_MODEL), 0.01),
        'rg_conv_w': nrm(ks[4], (L, RG_CONV, RG_WIDTH), RG_CONV ** -0.5),
        'rg_conv_b': nrm(ks[5], (L, RG_WIDTH), 0.01),
        'rg_w_a': nrm(ks[6], (L, RG_BLOCKS, RG_BLOCK_DIM, RG_BLOCK_DIM), RG_BLOCK_DIM ** -0.5),
        'rg_b_a': nrm(ks[7], (L, RG_WIDTH), 0.01),
        'rg_w_x': nrm(ks[8], (L, RG_BLOCKS, RG_BLOCK_DIM, RG_BLOCK_DIM), RG_BLOCK_DIM ** -0.5),
        'rg_b_x': nrm(ks[9], (L, RG_WIDTH), 0.01),
        'rg_lambda': jnp.log(a0) - jnp.log1p(-a0),
        'diff_lq1': nrm(ks[11], (L, DIFF_HEAD_DIM), 0.1),
        'diff_lk1': nrm(ks[12], (L, DIFF_HEAD_DIM), 0.1),
        'diff_lq2': nrm(ks[13], (L, DIFF_HEAD_DIM), 0.1),
        'diff_lk2': nrm(ks[14], (L, DIFF_HEAD_DIM), 0.1),
        'diff_subln_g': gain(ks[15], (L, 2 * DIFF_HEAD_DIM)),
        'w_branch_a': nrm(ks[16], (L, MOBA_WIDTH, D_MODEL), MOBA_WIDTH ** -0.5),
        'w_branch_b': nrm(ks[17], (L, RG_WIDTH, D_MODEL), RG_WIDTH ** -0.5),
        'w_branch_c': nrm(ks[18], (L, DIFF_V_WIDTH, D_MODEL), DIFF_V_WIDTH ** -0.5),
        'w_out': nrm(ks[19], (L, D_MODEL, D_MODEL), D_MODEL ** -0.5),
        'norm_ffn_g': gain(ks[20], (L, D_MODEL)),
        'ffn_up': nrm(ks[21], (L, D_MODEL, 2 * D_FF), D_MODEL ** -0.5),
        'ffn_conv_w': nrm(ks[22], (L, FFN_CONV, 2 * D_FF), FFN_CONV ** -0.5),
        'ffn_conv_b': nrm(ks[23], (L, 2 * D_FF), 0.01),
        'ffn_down': nrm(ks[24], (L, D_FF, D_MODEL), D_FF ** -0.5),
        'final_norm_g': gain(ks[25], (D_MODEL,)),
    }


def reference(x, norm_mix_g, w_in, b_gate, rg_conv_w, rg_conv_b, rg_w_a, rg_b_a, rg_w_x, rg_b_x,
              rg_lambda, diff_lq1, diff_lk1, diff_lq2, diff_lk2, diff_subln_g, w_branch_a, w_branch_b,
              w_branch_c, w_out, norm_ffn_g, ffn_up, ffn_conv_w, ffn_conv_b, ffn_down, final_norm_g):
    B, S = x.shape[0], x.shape[1]
    offs = split_offsets()
    for l in range(DEPTH):
        h = rms_norm(x, norm_mix_g[l])
        proj = h @ w_in[l]
        qa, ka, va, qc, kc, vc, rx, rgate, gates = jnp.split(proj, offs, axis=-1)
        ya = moba_attention(qa.reshape(B, S, MOBA_HEADS, MOBA_HEAD_DIM),
                            ka.reshape(B, S, MOBA_HEADS, MOBA_HEAD_DIM),
                            va.reshape(B, S, MOBA_HEADS, MOBA_HEAD_DIM)).reshape(B, S, MOBA_WIDTH)
        xr = causal_dwconv(rx, rg_conv_w[l], rg_conv_b[l])
        yb = rg_lru(xr, rg_w_a[l], rg_b_a[l], rg_w_x[l], rg_b_x[l], rg_lambda[l]) * jax.nn.gelu(rgate)
        lam_init = 0.8 - 0.6 * math.exp(-0.3 * l)
        lam = (jnp.exp(jnp.sum(diff_lq1[l].astype(jnp.float32) * diff_lk1[l].astype(jnp.float32)))
               - jnp.exp(jnp.sum(diff_lq2[l].astype(jnp.float32) * diff_lk2[l].astype(jnp.float32)))
               + lam_init)
        yc = diff_attention(qc.reshape(B, S, DIFF_HEADS, 2, DIFF_HEAD_DIM),
                            kc.reshape(B, S, DIFF_HEADS, 2, DIFF_HEAD_DIM),
                            vc.reshape(B, S, DIFF_HEADS, 2 * DIFF_HEAD_DIM),
                            lam, lam_init, diff_subln_g[l])
        g = jax.nn.sigmoid(gates + b_gate[l]).reshape(B, S, N_BRANCHES, D_MODEL)
        merged = (g[:, :, 0] * (ya @ w_branch_a[l])
                  + g[:, :, 1] * (yb @ w_branch_b[l])
                  + g[:, :, 2] * (yc @ w_branch_c[l]))
        x = x + merged @ w_out[l]
        h = rms_norm(x, norm_ffn_g[l])
        u = causal_dwconv(h @ ffn_up[l], ffn_conv_w[l], ffn_conv_b[l])
        u_gate, u_val = jnp.split(u, 2, axis=-1)
        x = x + (jax.nn.gelu(u_gate) * u_val) @ ffn_down[l]
    return rms_norm(x, final_norm_g)
```

```python
import contextlib
import numpy as np
import ml_dtypes
import concourse.bass as bass
import concourse.mybir as mybir
from concourse.bass_utils import run_bass_kernel_spmd

F32 = mybir.dt.float32
BF16 = mybir.dt.bfloat16
AF = mybir.ActivationFunctionType
ALU = mybir.AluOpType
AX = mybir.AxisListType

T = 4096
D = 1024
NG = 8
NT = 32
DFF = 2816
NFC = 22
EPS = 1e-6
BIG = 32768.0
ALIBI_CUT = 48.0
MOBA_SLOPES = [2.0 ** (-(h + 1)) for h in range(8)]
DIFF_SLOPES = [2.0 ** (-2.0 * (h + 1)) for h in range(4)]
NPV = 281
O_QA, O_KA, O_VA, O_QC, O_KC, O_VC, O_RX, O_RG, O_GT = 0, 512, 1024, 1536, 2048, 2560, 3072, 4096, 5120
PV_GMIX, PV_GFFN, PV_BGATE, PV_RCW, PV_RCB, PV_RBA, PV_RBX, PV_RLAM, PV_SUBG, PV_FCW, PV_FCB = \
    0, 8, 16, 40, 72, 80, 88, 96, 104, 105, 237


class Tile:
    def __init__(self, kb, t, name):
        self.kb = kb
        self.t = t
        self.name = name
        self.w = None
        self.r = []
        self.dsem = None
        self.dcnt = 0
        self.is_psum = False

    def __getitem__(self, idx):
        return self.t[idx]


class KB:
    def __init__(self, NL=4, debug=False, stop_after=None):
        self.NL = NL
        self.debug = debug
        self.stop_after = stop_after
        nc = self.nc = bass.Bass("TRN2", target_bir_lowering=False)
        self.E = {'pe': nc.tensor, 'act': nc.scalar, 'dve': nc.vector, 'pool': nc.gpsimd, 'sp': nc.sync}
        self.cnt = {e: 0 for e in self.E}
        self.seen = {e: {} for e in self.E}
        self.top = contextlib.ExitStack()
        self.sem = {e: self.top.enter_context(nc.semaphore("s_" + e)) for e in self.E}
        self.bar_sem = self.top.enter_context(nc.semaphore("s_bar"))
        self.nbar = 0
        self.dsem_free = [self.top.enter_context(nc.semaphore("s_d%d" % i)) for i in range(64)]
        self.dsem_cnt = {}
        self.phase = None
        self.phase_tiles = []
        self.persist_tiles = []
        self.uid = 0

    def _mk(self, es, lst, name, shape, dt, psum=False):
        self.uid += 1
        nm = "%s_%d" % (name, self.uid)
        if psum:
            t = es.enter_context(self.nc.psum_tensor(nm, shape, dt))
        else:
            t = es.enter_context(self.nc.sbuf_tensor(nm, shape, dt))
        tl = Tile(self, t, nm)
        tl.is_psum = psum
        lst.append(tl)
        return tl

    def ptile(self, name, shape, dt):
        return self._mk(self.top, self.persist_tiles, name, shape, dt)

    def tile(self, name, shape, dt):
        return self._mk(self.phase, self.phase_tiles, name, shape, dt)

    def psum(self, name, shape=None, dt=F32):
        return self._mk(self.phase, self.phase_tiles, name, shape or [128, 512], dt, psum=True)

    def begin_phase(self):
        self.phase = contextlib.ExitStack()
        self.phase_tiles = []

    def end_phase(self):
        self.barrier()
        for tl in self.phase_tiles:
            if tl.dsem is not None:
                self.dsem_free.append(tl.dsem)
                tl.dsem = None
        self.phase.close()
        self.phase = None
        self.phase_tiles = []

    def _wait(self, e, evs):
        eng = self.E[e]
        need = {}
        for (sem, val, owner) in evs:
            if owner is not None and owner.dsem is sem:
                val = max(val, owner.dcnt)
            if val > need.get(sem, (0, None))[0]:
                need[sem] = (val, sem)
        for sem, (val, _) in need.items():
            if self.seen[e].get(sem, 0) >= val:
                continue
            if sem is self.sem[e] and (e == 'pe' or val <= self.cnt[e] - 4):
                continue
            eng.wait_ge(sem, val)
            self.seen[e][sem] = val

    @staticmethod
    def _gather(R, W):
        evs = []
        for r in R:
            if r.w is not None:
                evs.append(r.w)
            if r.is_psum:
                evs.extend(r.r)
        for w in W:
            if w.w is not None:
                evs.append(w.w)
            evs.extend(w.r)
        return evs

    @staticmethod
    def _record(ev, R, W):
        for r in R:
            r.r = [x for x in r.r if x[0] is not ev[0]] + [ev]
        for w in W:
            w.w = ev
            w.r = []

    def op(self, e, fn, R=(), W=()):
        self._wait(e, self._gather(R, W))
        inst = fn()
        self.cnt[e] += 1
        inst.then_inc(self.sem[e], 1)
        ev = (self.sem[e], self.cnt[e], None)
        self._record(ev, R, W)
        return inst

    def dma(self, q, out, in_, R=(), W=(), owner=None):
        self._wait(q, self._gather(R, W))
        if owner.dsem is None:
            owner.dsem = self.dsem_free.pop()
            owner.dcnt = self.dsem_cnt.get(owner.dsem, 0)
        inst = self.E[q].dma_start(out=out, in_=in_)
        owner.dcnt += 16
        self.dsem_cnt[owner.dsem] = owner.dcnt
        inst.then_inc(owner.dsem, 16)
        ev = (owner.dsem, owner.dcnt, owner)
        self._record(ev, R, W)
        return inst

    def barrier(self):
        sp = self.E['sp']
        for e in self.E:
            if e == 'sp' or self.cnt[e] == 0:
                continue
            if self.seen['sp'].get(self.sem[e], 0) < self.cnt[e]:
                sp.wait_ge(self.sem[e], self.cnt[e])
                self.seen['sp'][self.sem[e]] = self.cnt[e]
        for sem, c in self.dsem_cnt.items():
            if self.seen['sp'].get(sem, 0) < c:
                sp.wait_ge(sem, c)
                self.seen['sp'][sem] = c
        self.nbar += 1
        sp.sem_inc(self.bar_sem, 1)
        for e in self.E:
            if e == 'sp':
                continue
            self.E[e].wait_ge(self.bar_sem, self.nbar)
            for e2 in self.E:
                self.seen[e][self.sem[e2]] = self.cnt[e2]
            for sem, c in self.dsem_cnt.items():
                self.seen[e][sem] = c
        for e2 in self.E:
            self.seen['sp'][self.sem[e2]] = self.cnt[e2]
        for tl in self.phase_tiles + self.persist_tiles:
            tl.w = None
            tl.r = []

    def dram(self, name, shape, dt, kind="Internal"):
        if self.debug and kind == "Internal":
            kind = "ExternalOutput"
        return self.nc.dram_tensor(name, shape, dt, kind=kind).ap()

    def declare(self):
        NL = self.NL
        d = self.dram
        ei = "ExternalInput"
        self.x_in = d("x", [T, D], F32, ei)
        self.w_in = d("w_in", [4, D, 8192], F32, ei)
        self.w_a = d("w_branch_a", [4, 512, D], F32, ei)
        self.w_b = d("w_branch_b", [4, D, D], F32, ei)
        self.w_c = d("w_branch_c", [4, 512, D], F32, ei)
        self.w_o = d("w_out", [4, D, D], F32, ei)
        self.f_up = d("ffn_up", [4, D, 2 * DFF], F32, ei)
        self.f_dn = d("ffn_down", [4, DFF, D], F32, ei)
        self.pvec_d = d("pvec", [128, 4 * NPV], F32, ei)
        self.bd_d = d("bdiag", [4, 2, 8, 128, 128], F32, ei)
        self.lqk_d = d("lqk", [128, 4 * 4 * 64], F32, ei)
        self.gfin_d = d("gfin", [128, D], F32, ei)
        self.cbf_d = d("cbf", [128, 256], F32, ei)
        self.qpos_d = d("qpos", [6, T], F32, ei)
        self.kpos_d = d("kpos", [12, 6, T], F32, ei)
        self.koh_d = d("koh", [16, T], F32, ei)
        self.gtab_d = d("gtab", [128, 2 * 32 * 16], F32, ei)
        self.out = d("out", [T, D], F32, "ExternalOutput")
        self.xA = d("xA", [T, D], F32)
        self.xB = d("xB", [T, D], F32)
        self.qaT = d("qaT", [512, T], BF16)
        self.kaT = d("kaT", [512, T], BF16)
        self.va = d("va", [T, 512], BF16)
        self.qcT = d("qcT", [512, T], BF16)
        self.kcT = d("kcT", [512, T], BF16)
        self.vc = d("vc", [T, 512], BF16)
        self.gT = d("gT", [3072, T], BF16)
        self.yaT = d("yaT", [512, T], BF16)
        self.ybT = d("ybT", [D, T], BF16)
        self.ycT = d("ycT", [512, T], BF16)
        self.aT = d("aT", [DFF, T], BF16)

    def setup(self):
        nc = self.nc
        NL = self.NL
        self.pvec = self.ptile("pvec", [128, 4 * NPV], F32)
        self.cbf = self.ptile("cbf", [128, 256], BF16)
        self.ones_bf = self.ptile("ones_bf", [128, 128], BF16)
        self.ones_f = self.ptile("ones_f", [128, 128], F32)
        self.eps_t = self.ptile("eps", [128, 1], F32)
        self.one_t = self.ptile("one", [128, 1], F32)
        self.zero_t = self.ptile("zero", [128, 1], F32)
        self.sA = self.ptile("sA", [128, 4 * 8], F32)
        self.sA2 = self.ptile("sA2", [128, 4 * 8], F32)
        self.gsub = self.ptile("gsub", [128, 4], F32)
        self.nlam = self.ptile("nlam", [128, 4], F32)
        self.begin_phase()
        self.dma('sp', self.pvec[:], self.pvec_d[:, :], W=[self.pvec], owner=self.pvec)
        self.dma('pool', self.cbf[:], self.cbf_d[:, :], W=[self.cbf], owner=self.cbf)
        self.ident = self.cbf
        self.op('dve', lambda: nc.vector.memset(self.ones_bf[:], 1.0), W=[self.ones_bf])
        self.op('dve', lambda: nc.vector.memset(self.ones_f[:], 1.0), W=[self.ones_f])
        self.op('dve', lambda: nc.vector.memset(self.eps_t[:], EPS), W=[self.eps_t])
        self.op('dve', lambda: nc.vector.memset(self.one_t[:], 1.0), W=[self.one_t])
        self.op('dve', lambda: nc.vector.memset(self.zero_t[:], 0.0), W=[self.zero_t])
        tmp = self.tile("tmp_sa", [128, 4 * 8], F32)
        pv4 = self.pvec[:, :].rearrange("p (l c) -> p l c", l=4)
        lam_v = pv4[:, :, PV_RLAM:PV_RLAM + 8]
        tmp_v = tmp[:, :].rearrange("p (l c) -> p l c", l=4)
        self.op('act', lambda: nc.scalar.activation(out=tmp_v, in_=lam_v, func=AF.Exp, scale=-1.0), R=[self.pvec], W=[tmp])
        self.op('act', lambda: nc.scalar.activation(out=tmp[:], in_=tmp[:], func=AF.Ln, bias=self.one_t[:, 0:1], scale=1.0), R=[tmp, self.one_t], W=[tmp])
        self.op('dve', lambda: nc.vector.tensor_scalar_mul(self.sA[:], tmp[:], -8.0), R=[tmp], W=[self.sA])
        self.op('dve', lambda: nc.vector.tensor_scalar_mul(self.sA2[:], tmp[:], -16.0), R=[tmp], W=[self.sA2])
        import math
        self.lam_init = [0.8 - 0.6 * math.exp(-0.3 * l) for l in range(4)]
        for l in range(4):
            self.op('dve', lambda: nc.vector.tensor_scalar_mul(self.gsub[:, l:l + 1], self.pvec[:, l * NPV + PV_SUBG:l * NPV + PV_SUBG + 1], 1.0 - self.lam_init[l]), R=[self.pvec], W=[self.gsub])
        lqk = self.tile("lqk", [128, 4 * 4 * 64], F32)
        self.dma('sp', lqk[:], self.lqk_d[:, :], W=[lqk], owner=lqk)
        prod = self.tile("lprod", [128, 4 * 2 * 64], F32)
        lv = lqk[:, :].rearrange("p (l w d) -> p l w d", l=4, w=4)
        pvw = prod[:, :].rearrange("p (l w d) -> p l w d", l=4, w=2)
        self.op('dve', lambda: nc.vector.tensor_tensor(pvw[:, :, 0, :], lv[:, :, 0, :], lv[:, :, 1, :], ALU.mult), R=[lqk], W=[prod])
        self.op('dve', lambda: nc.vector.tensor_tensor(pvw[:, :, 1, :], lv[:, :, 2, :], lv[:, :, 3, :], ALU.mult), R=[lqk], W=[prod])
        ssum = self.tile("lsum", [128, 8], F32)
        self.op('dve', lambda: nc.vector.tensor_reduce(out=ssum[:, :], in_=prod[:, :].rearrange("p (a d) -> p a d", d=64), axis=AX.X, op=ALU.add), R=[prod], W=[ssum])
        self.op('act', lambda: nc.scalar.activation(out=ssum[:], in_=ssum[:], func=AF.Exp), R=[ssum], W=[ssum])
        sv = ssum[:, :].rearrange("p (l w) -> p l w", w=2)
        self.op('dve', lambda: nc.vector.tensor_tensor(self.nlam[:, :], sv[:, :, 1], sv[:, :, 0], ALU.subtract), R=[ssum], W=[self.nlam])
        for l in range(4):
            self.op('dve', lambda: nc.vector.tensor_scalar_add(self.nlam[:, l:l + 1], self.nlam[:, l:l + 1], -self.lam_init[l]), R=[self.nlam], W=[self.nlam])
        self.end_phase()

    def norm_transpose(self, xsrc, gcol0, hT):
        nc = self.nc
        xp = [self.tile("nx", [128, D], F32) for _ in range(3)]
        xn = [self.tile("nxn", [128, D], BF16) for _ in range(2)]
        junk = self.tile("njunk", [128, D], BF16)
        ss = [self.tile("nss", [128, 1], F32) for _ in range(3)]
        rs = [self.tile("nrs", [128, 1], F32) for _ in range(3)]
        pt = [self.psum("npt") for _ in range(2)]
        self.npt = pt
        for tt in range(NT):
            x_t = xp[tt % 3]
            xn_t = xn[tt % 2]
            ss_t = ss[tt % 3]
            rs_t = rs[tt % 3]
            self.dma('sp', x_t[:], xsrc[tt * 128:(tt + 1) * 128, :], W=[x_t], owner=x_t)
            self.op('dve', lambda: nc.vector.memset(ss_t[:], 0.0), W=[ss_t])
            self.op('act', lambda: nc.scalar.activation(out=junk[:], in_=x_t[:], func=AF.Square, accum_out=ss_t[:]), R=[x_t, ss_t], W=[junk, ss_t])
            self.op('act', lambda: nc.scalar.activation(out=rs_t[:], in_=ss_t[:], func=AF.Sqrt, scale=1.0 / D, bias=self.eps_t[:, 0:1]), R=[ss_t, self.eps_t], W=[rs_t])
            self.op('dve', lambda: nc.vector.reciprocal(rs_t[:], rs_t[:]), R=[rs_t], W=[rs_t])
            self.op('dve', lambda: nc.vector.tensor_scalar_mul(xn_t[:], x_t[:], rs_t[:, 0:1]), R=[x_t, rs_t], W=[xn_t])
            for half in range(2):
                p_t = pt[half]
                for c4 in range(4):
                    c = half * 4 + c4
                    self.op('pe', lambda: nc.tensor.matmul(p_t[:, c4 * 128:(c4 + 1) * 128], xn_t[:, c * 128:(c + 1) * 128], self.ident[:, 0:128], start=True, stop=True), R=[xn_t, self.ident], W=[p_t])
                gv = self.pvec[:, gcol0 + half * 4:gcol0 + half * 4 + 4].unsqueeze(2).broadcast_to([128, 4, 128])
                self.op('dve', lambda: nc.vector.tensor_tensor(hT[:, half * 4:half * 4 + 4, tt * 128:(tt + 1) * 128], p_t[:, :].rearrange("p (c t) -> p c t", c=4), gv, ALU.mult), R=[p_t, self.pvec], W=[hT])

    def phase1(self, l):
        nc = self.nc
        self.begin_phase()
        xsrc = self.x_in if l == 0 else self.xA
        pb = l * NPV
        hT = self.tile("hT", [128, 8, T], BF16)
        self.norm_transpose(xsrc, pb + PV_GMIX, hT)
        wv = self.w_in[l].rearrange("(kc p) n -> p kc n", p=128)
        wb = [self.tile("wb", [128, 8, 512], BF16) for _ in range(3)]
        st = [self.tile("st", [128, 4, 512], BF16) for _ in range(3)]
        ps = [self.psum("p1ps") for _ in range(4)]
        jobs = [(O_QA, self.qaT, 0, None, 'f'), (O_KA, self.kaT, 0, None, 'f'), (O_QC, self.qcT, 0, None, 'f'), (O_KC, self.kcT, 0, None, 'f')]
        for i in range(6):
            jobs.append((O_GT + i * 512, self.gT, i * 512, pb + PV_BGATE + i * 4, 'f'))
        jobs.append((O_VA, self.va, 0, None, 't'))
        jobs.append((O_VC, self.vc, 0, None, 't'))
        cnts = {'ps': 0, 'st': 0}

        def load_w(ji):
            c0 = jobs[ji][0]
            w_t = wb[ji % 3]
            self.dma('pool', w_t[:], wv[:, :, c0:c0 + 512], W=[w_t], owner=w_t)

        def units():
            load_w(0)
            for ji, (c0, dst, r0, bcol, kind) in enumerate(jobs):
                w_t = wb[ji % 3]
                if ji + 1 < len(jobs):
                    load_w(ji + 1)
                if kind == 'f':
                    dview = dst[r0:r0 + 512, :].rearrange("(m p) t -> p m t", p=128)
                    for g in range(NG):
                        s_t = st[cnts['st'] % 3]
                        cnts['st'] += 1
                        for m in range(4):
                            p_t = ps[cnts['ps'] % 4]
                            cnts['ps'] += 1
                            for kc in range(8):
                                self.op('pe', lambda: nc.tensor.matmul(p_t[:, :], w_t[:, kc, m * 128:(m + 1) * 128], hT[:, kc, g * 512:(g + 1) * 512], start=(kc == 0), stop=(kc == 7)), R=[w_t, hT], W=[p_t])
                            if bcol is not None:
                                self.op('act', lambda: nc.scalar.activation(out=s_t[:, m, :], in_=p_t[:, :], func=AF.Sigmoid, bias=self.pvec[:, bcol + m:bcol + m + 1], scale=1.0), R=[p_t, self.pvec], W=[s_t])
                            elif m % 2 == 0:
                                self.op('act', lambda: nc.scalar.copy(s_t[:, m, :], p_t[:, :]), R=[p_t], W=[s_t])
                            else:
                                self.op('dve', lambda: nc.vector.tensor_copy(s_t[:, m, :], p_t[:, :]), R=[p_t], W=[s_t])
                            if m == 3:
                                self.dma('sp', dview[:, :, g * 512:(g + 1) * 512], s_t[:], R=[s_t], owner=s_t)
                            yield
                else:
                    for tt in range(NT):
                        p_t = ps[cnts['ps'] % 4]
                        cnts['ps'] += 1
                        s_t = st[cnts['st'] % 3]
                        cnts['st'] += 1
                        for kc in range(8):
                            self.op('pe', lambda: nc.tensor.matmul(p_t[:, :], hT[:, kc, tt * 128:(tt + 1) * 128], w_t[:, kc, :], start=(kc == 0), stop=(kc == 7)), R=[w_t, hT], W=[p_t])
                        if tt % 2 == 0:
                            self.op('act', lambda: nc.scalar.copy(s_t[:, 0, :], p_t[:, :]), R=[p_t], W=[s_t])
                        else:
                            self.op('dve', lambda: nc.vector.tensor_copy(s_t[:, 0, :], p_t[:, :]), R=[p_t], W=[s_t])
                        self.dma('sp', dst[tt * 128:(tt + 1) * 128, :], s_t[:, 0, :], R=[s_t], owner=s_t)
                        yield

        wr = [self.tile("wr", [128, 8, 256], BF16) for _ in range(2)]
        bd = [self.tile("bd", [128, 2, 128], BF16) for _ in range(2)]
        rxs = [self.tile("rxs", [128, 515], F32) for _ in range(2)]
        xr = [self.tile("xr", [128, 512], F32) for _ in range(2)]
        xrb = [self.tile("xrb", [128, 512], BF16) for _ in range(2)]
        rr = [self.tile("rr", [128, 512], F32) for _ in range(2)]
        ii = [self.tile("ii", [128, 512], F32) for _ in range(2)]
        aa = [self.tile("aa", [128, 512], F32) for _ in range(2)]
        a2 = [self.tile("a2", [128, 512], F32) for _ in range(2)]
        bb = [self.tile("bb", [128, 512], F32) for _ in range(2)]
        hs = [self.tile("hs", [128, 512], F32) for _ in range(2)]
        gl = [self.tile("gl", [128, 512], F32) for _ in range(2)]
        yb = [self.tile("yb", [128, 512], BF16) for _ in range(3)]
        pa = self.psum("p1pa")
        px = self.psum("p1px")
        prx, prg = self.npt[0], self.npt[1]

        def load_r(j):
            w_t = wr[j % 2]
            b_t = bd[j % 2]
            self.dma('pool', w_t[:, :, 0:128], wv[:, :, O_RX + j * 128:O_RX + (j + 1) * 128], W=[w_t], owner=w_t)
            self.dma('pool', w_t[:, :, 128:256], wv[:, :, O_RG + j * 128:O_RG + (j + 1) * 128], W=[w_t], owner=w_t)
            self.dma('pool', b_t[:, 0, :], self.bd_d[l, 0, j], W=[b_t], owner=b_t)
            self.dma('pool', b_t[:, 1, :], self.bd_d[l, 1, j], W=[b_t], owner=b_t)

        def rg():
            it = 0
            load_r(0)
            for j in range(8):
                w_t = wr[j % 2]
                b_t = bd[j % 2]
                cw = lambda i: self.pvec[:, pb + PV_RCW + i * 8 + j:pb + PV_RCW + i * 8 + j + 1]
                cb = self.pvec[:, pb + PV_RCB + j:pb + PV_RCB + j + 1]
                ba = self.pvec[:, pb + PV_RBA + j:pb + PV_RBA + j + 1]
                bx = self.pvec[:, pb + PV_RBX + j:pb + PV_RBX + j + 1]
                sa = self.sA[:, l * 8 + j:l * 8 + j + 1]
                sa2 = self.sA2[:, l * 8 + j:l * 8 + j + 1]
                for g in range(NG):
                    k = it % 2
                    it += 1
                    for kc in range(8):
                        self.op('pe', lambda: nc.tensor.matmul(prx[:, :], w_t[:, kc, 0:128], hT[:, kc, g * 512:(g + 1) * 512], start=(kc == 0), stop=(kc == 7)), R=[w_t, hT], W=[prx])
                    for kc in range(8):
                        self.op('pe', lambda: nc.tensor.matmul(prg[:, :], w_t[:, kc, 128:256], hT[:, kc, g * 512:(g + 1) * 512], start=(kc == 0), stop=(kc == 7)), R=[w_t, hT], W=[prg])
                    if g == NG - 1 and j + 1 < 8:
                        load_r(j + 1)
                    rx_t, xr_t, xrb_t = rxs[k], xr[k], xrb[k]
                    if g == 0:
                        self.op('dve', lambda: nc.vector.memset(rx_t[:, 0:3], 0.0), W=[rx_t])
                    else:
                        prev = rxs[1 - k]
                        self.op('dve', lambda: nc.vector.tensor_copy(rx_t[:, 0:3], prev[:, 512:515]), R=[prev], W=[rx_t])
                    self.op('act', lambda: nc.scalar.copy(rx_t[:, 3:515], prx[:, :]), R=[prx], W=[rx_t])
                    r_t, i_t, a_t, a2_t, b_t2, h_t, g_t = rr[k], ii[k], aa[k], a2[k], bb[k], hs[k], gl[k]
                    self.op('act', lambda: nc.scalar.activation(out=g_t[:], in_=prg[:, :], func=AF.Gelu_apprx_tanh), R=[prg], W=[g_t])
                    self.op('dve', lambda: nc.vector.tensor_scalar(xr_t[:], rx_t[:, 3:515], cw(3), cb, ALU.mult, ALU.add), R=[rx_t, self.pvec], W=[xr_t])
                    for i in range(3):
                        self.op('dve', lambda: nc.vector.scalar_tensor_tensor(xr_t[:], rx_t[:, i:i + 512], cw(i), xr_t[:], ALU.mult, ALU.add), R=[rx_t, self.pvec, xr_t], W=[xr_t])
                    self.op('act', lambda: nc.scalar.copy(xrb_t[:], xr_t[:]), R=[xr_t], W=[xrb_t])
                    yield
                    self.op('pe', lambda: nc.tensor.matmul(pa[:, :], b_t[:, 0, :], xrb_t[:], start=True, stop=True), R=[b_t, xrb_t], W=[pa])
                    self.op('pe', lambda: nc.tensor.matmul(px[:, :], b_t[:, 1, :], xrb_t[:], start=True, stop=True), R=[b_t, xrb_t], W=[px])
                    self.op('act', lambda: nc.scalar.activation(out=r_t[:], in_=pa[:, :], func=AF.Sigmoid, bias=ba, scale=1.0), R=[pa, self.pvec], W=[r_t])
                    self.op('act', lambda: nc.scalar.activation(out=i_t[:], in_=px[:, :], func=AF.Sigmoid, bias=bx, scale=1.0), R=[px, self.pvec], W=[i_t])
                    self.op('act', lambda: nc.scalar.activation(out=a_t[:], in_=r_t[:], func=AF.Exp, scale=sa), R=[r_t, self.sA], W=[a_t])
                    self.op('act', lambda: nc.scalar.activation(out=a2_t[:], in_=r_t[:], func=AF.Exp, scale=sa2), R=[r_t, self.sA2], W=[a2_t])
                    self.op('act', lambda: nc.scalar.activation(out=a2_t[:], in_=a2_t[:], func=AF.Ln, scale=-1.0, bias=self.one_t[:, 0:1]), R=[a2_t, self.one_t], W=[a2_t])
                    self.op('act', lambda: nc.scalar.activation(out=a2_t[:], in_=a2_t[:], func=AF.Exp, scale=0.5), R=[a2_t], W=[a2_t])
                    self.op('dve', lambda: nc.vector.tensor_tensor(b_t2[:], i_t[:], xr_t[:], ALU.mult), R=[i_t, xr_t], W=[b_t2])
                    self.op('dve', lambda: nc.vector.tensor_tensor(b_t2[:], b_t2[:], a2_t[:], ALU.mult), R=[b_t2, a2_t], W=[b_t2])
                    if g == 0:
                        self.op('dve', lambda: nc.vector.tensor_tensor_scan(h_t[:], a_t[:], b_t2[:], 0.0, ALU.mult, ALU.add), R=[a_t, b_t2], W=[h_t])
                    else:
                        hp = hs[1 - k]
                        self.op('dve', lambda: nc.vector.tensor_tensor_scan(h_t[:], a_t[:], b_t2[:], hp[:, 511:512], ALU.mult, ALU.add), R=[a_t, b_t2, hp], W=[h_t])
                    y_t = yb[it % 3]
                    self.op('dve', lambda: nc.vector.tensor_tensor(y_t[:], h_t[:], g_t[:], ALU.mult), R=[h_t, g_t], W=[y_t])
                    self.dma('sp', self.ybT[j * 128:(j + 1) * 128, g * 512:(g + 1) * 512], y_t[:], R=[y_t], owner=y_t)
                    yield

        gu, gr = units(), rg()
        alive_u = alive_r = True
        while alive_u or alive_r:
            for _ in range(3):
                if alive_u:
                    try:
                        next(gu)
                    except StopIteration:
                        alive_u = False
            if alive_r:
                try:
                    next(gr)
                except StopIteration:
                    alive_r = False
        self.end_phase()

    def attn_tiles(self, g, slope):
        res = []
        for kt in range(4 * g + 4):
            j = kt - 4 * g
            dmin = g * 512 - (kt * 128 + 127)
            if j < 0 and slope * dmin >= ALIBI_CUT:
                continue
            res.append((kt, max(j, 0) * 128, j >= 0))
        return res

    def attn_group(self, Kt, Qt, KR, g, slope, pS, pT, emit_pv):
        nc = self.nc
        tiles = self.attn_tiles(g, slope)
        trimask = self.cbf
        LA = 2
        self.new_call()

        def emit_s(i):
            kt, c0, diag = tiles[i]
            p_t = pS[self.n_s % len(pS)]
            self.n_s += 1
            self.op('pe', lambda: nc.tensor.matmul(p_t[:, c0:512], Kt[0:KR, kt * 128:(kt + 1) * 128], Qt[0:KR, g * 512 + c0:(g + 1) * 512], start=True, stop=not diag), R=[Kt, Qt], W=[p_t])
            if diag:
                self.op('pe', lambda: nc.tensor.matmul(p_t[:, c0:c0 + 128], self.ident[:, 0:128], trimask[:, 128:256], start=False, stop=True), R=[self.cbf], W=[p_t])
            return p_t

        pend = [emit_s(i) for i in range(min(LA, len(tiles)))]
        for i in range(len(tiles)):
            kt, c0, diag = tiles[i]
            p_t = pend.pop(0)
            if i + LA < len(tiles):
                pend.append(emit_s(i + LA))
            self.tick()
            e_t = pT[self.n_e % len(pT)]
            self.n_e += 1
            self.op('act', lambda: nc.scalar.activation(out=e_t[:, c0:512], in_=p_t[:, c0:512], func=AF.Exp, scale=0.125), R=[p_t], W=[e_t])
            emit_pv(i, kt, c0, e_t, i == 0, i == len(tiles) - 1)

    def defer(self, ticks, fn):
        self.epi_q.append([ticks, fn, self.call_id])

    def new_call(self):
        self.call_id += 1
        while self.epi_q and self.epi_q[0][2] <= self.call_id - 2:
            self.epi_q.pop(0)[1]()

    def tick(self):
        self.n_tick += 1
        if self.bg_q and self.n_tick % 2 == 0:
            self.bg_q.pop(0)()
        for it in self.epi_q:
            it[0] -= 1
        while self.epi_q and self.epi_q[0][0] <= 0:
            self.epi_q.pop(0)[1]()

    def flush_epi(self):
        while self.epi_q:
            self.epi_q.pop(0)[1]()

    def phase2(self, l):
        nc = self.nc
        self.begin_phase()
        KR = 86
        Vall = self.tile("Vall", [128, NT, 512], BF16)
        self.dma('sp', Vall[:, 0:16, :], self.va.rearrange("(kt p) c -> p kt c", p=128)[:, 0:16, :], W=[Vall], owner=Vall)
        self.dma('sp', Vall[:, 16:32, :], self.va.rearrange("(kt p) c -> p kt c", p=128)[:, 16:32, :], W=[Vall], owner=Vall)
        gtab = self.tile("gtab", [128, 2 * 32 * 16], F32)
        self.dma('sp', gtab[:], self.gtab_d[:, :], W=[gtab], owner=gtab)
        pastneg = gtab[:, 0:512].rearrange("p (q n) -> p q n", n=16)
        Cq = gtab[:, 512:1024].rearrange("p (q n) -> p q n", n=16)
        Qa = [self.tile("Qa", [KR, T], BF16) for _ in range(2)]
        Ka = [self.tile("Ka", [KR, T], BF16) for _ in range(2)]
        Vh = [self.tile("Vh", [128, NT, 65], BF16) for _ in range(2)]
        q32 = self.tile("q32", [64, T], F32)
        kmean = self.tile("kmean", [64, 16], F32)
        gm = [self.tile("gm", [128, 4, 16], F32) for _ in range(2)]
        mx = [self.tile("mx", [128, 4, 8], F32) for _ in range(2)]
        thr = [self.tile("thr", [128, 4], F32) for _ in range(2)]
        sel = [self.tile("sel", [128, 4, 16], F32) for _ in range(2)]
        mb = [self.tile("mb", [128, 4, 80], BF16) for _ in range(2)]
        for m_t in mb:
            self.op('dve', lambda: nc.vector.memset(m_t[:], 0.0), W=[m_t])
        pT = [self.tile("pT", [128, 512], BF16) for _ in range(4)]
        osb = [self.tile("osb", [65, 512], F32) for _ in range(2)]
        rden = [self.tile("rden", [65, 512], F32) for _ in range(2)]
        yst = [self.tile("yst", [64, 512], BF16) for _ in range(3)]
        pS = [self.psum("pS") for _ in range(3)]
        pO = [self.psum("pO") for _ in range(2)]
        pG = self.psum("pG")
        pM = self.psum("pM")
        pB = self.psum("pB")
        self.n_s = 0
        self.n_e = 0
        self.epi_q = []
        self.bg_q = []
        self.n_tick = 0
        self.call_id = 0
        n_o = 0
        def load_head(h):
            Q_t, K_t, V_t = Qa[h % 2], Ka[h % 2], Vh[h % 2]
            self.dma('sp', Q_t[0:64, :], self.qaT[h * 64:(h + 1) * 64, :], W=[Q_t], owner=Q_t)
            self.dma('pool', Q_t[80:86, :], self.qpos_d[:, :], W=[Q_t], owner=Q_t)
            self.dma('sp', K_t[0:64, :], self.kaT[h * 64:(h + 1) * 64, :], W=[K_t], owner=K_t)
            self.dma('pool', K_t[64:80, :], self.koh_d[:, :], W=[K_t], owner=K_t)
            self.dma('pool', K_t[80:86, :], self.kpos_d[h], W=[K_t], owner=K_t)
            self.op('dve', lambda: nc.vector.tensor_copy(V_t[:, :, 0:64], Vall[:, :, h * 64:(h + 1) * 64]), R=[Vall], W=[V_t])
            self.op('dve', lambda: nc.vector.memset(V_t[:, :, 64:65], 1.0), W=[V_t])

        def gating_chunks(h):
            Q_t, K_t = Qa[h % 2], Ka[h % 2]
            chunks = []

            def c0():
                self.op('dve', lambda: nc.vector.tensor_copy(q32[:, :], Q_t[0:64, :]), R=[Q_t], W=[q32])
                self.op('dve', lambda: nc.vector.tensor_reduce(out=kmean[:, :], in_=K_t[0:64, :].rearrange("p (n k) -> p n k", k=256), axis=AX.X, op=ALU.add), R=[K_t], W=[kmean])
            chunks.append(c0)
            p1s, p2s = [], []
            for g in range(NG):
                def p1(g=g):
                    k = g % 2
                    gm_t, mx_t, thr_t, sel_t, mb_t = gm[k], mx[k], thr[k], sel[k], mb[k]
                    for s_ in range(4):
                        qt = 4 * g + s_
                        self.op('pe', lambda: nc.tensor.matmul(pG[:, s_ * 16:(s_ + 1) * 16], q32[0:64, qt * 128:(qt + 1) * 128], kmean[0:64, :], start=True, stop=True), R=[q32, kmean], W=[pG])
                    self.op('dve', lambda: nc.vector.tensor_tensor(gm_t[:], pG[:, 0:64].rearrange("p (s n) -> p s n", n=16), pastneg[:, 4 * g:4 * g + 4, :], ALU.add), R=[pG, gtab], W=[gm_t])
                    for s_ in range(4):
                        self.op('dve', lambda: nc.vector.max(mx_t[:, s_, :], gm_t[:, s_, :]), R=[gm_t], W=[mx_t])
                    self.op('dve', lambda: nc.vector.tensor_scalar_max(thr_t[:, :], mx_t[:, :, 2], -1e29), R=[mx_t], W=[thr_t])
                    self.op('dve', lambda: nc.vector.tensor_tensor(sel_t[:], gm_t[:], thr_t[:, :].unsqueeze(2).broadcast_to([128, 4, 16]), ALU.is_ge), R=[gm_t, thr_t], W=[sel_t])
                    self.op('dve', lambda: nc.vector.scalar_tensor_tensor(mb_t[:, :, 64:80], sel_t[:], BIG, Cq[:, 4 * g:4 * g + 4, :], ALU.mult, ALU.add), R=[sel_t, gtab], W=[mb_t])

                def p2(g=g):
                    mb_t = mb[g % 2]
                    for s_ in range(4):
                        self.op('pe', lambda: nc.tensor.matmul(pM[0:80, s_ * 128:(s_ + 1) * 128], mb_t[:, s_, :], self.ident[:, 0:128], start=True, stop=True), R=[mb_t, self.ident], W=[pM])
                    self.op('dve', lambda: nc.vector.tensor_copy(Q_t[64:80, g * 512:(g + 1) * 512], pM[64:80, :]), R=[pM], W=[Q_t])
                p1s.append(p1)
                p2s.append(p2)
            chunks.append(p1s[0])
            for g in range(1, NG):
                chunks.append(p1s[g])
                chunks.append(p2s[g - 1])
            chunks.append(p2s[NG - 1])
            return chunks

        load_head(0)
        for c in gating_chunks(0):
            c()
        for h in range(8):
            Q_t, K_t, V_t = Qa[h % 2], Ka[h % 2], Vh[h % 2]
            if h + 1 < 8:
                load_head(h + 1)
                self.bg_q = gating_chunks(h + 1)
            steep = len(self.attn_tiles(4, MOBA_SLOPES[h])) <= 10
            for g in range(NG):
                o_t = pO[n_o % 2]
                os_t = osb[n_o % 2]
                rd_t = rden[n_o % 2]
                n_o += 1

                def pv(i, kt, c0, e_t, first, last):
                    self.op('pe', lambda: nc.tensor.matmul(o_t[0:65, c0:512], V_t[:, kt, :], e_t[:, c0:512], start=first, stop=last), R=[V_t, e_t], W=[o_t])

                self.attn_group(K_t, Q_t, KR, g, MOBA_SLOPES[h], pS, pT, pv)
                y_t = yst[n_o % 3]

                def epiA(o_t=o_t, os_t=os_t):
                    self.op('act', lambda: nc.scalar.copy(os_t[0:65, :], o_t[0:65, :]), R=[o_t], W=[os_t])
                    self.op('pe', lambda: nc.tensor.matmul(pB[0:64, :], self.ones_f[64:65, 0:64], os_t[64:65, :], start=True, stop=True), R=[os_t, self.ones_f], W=[pB])

                def epiB(os_t=os_t, rd_t=rd_t, y_t=y_t, h=h, g=g, steep=steep):
                    if steep:
                        self.op('act', lambda: nc.scalar.activation(out=rd_t[0:64, :], in_=pB[0:64, :], func=AF.Ln), R=[pB], W=[rd_t])
                        self.op('act', lambda: nc.scalar.activation(out=rd_t[0:64, :], in_=rd_t[0:64, :], func=AF.Exp, scale=-1.0), R=[rd_t], W=[rd_t])
                    else:
                        self.op('dve', lambda: nc.vector.reciprocal(rd_t[0:64, :], pB[0:64, :]), R=[pB], W=[rd_t])
                    self.op('dve', lambda: nc.vector.tensor_tensor(y_t[:, :], os_t[0:64, :], rd_t[0:64, :], ALU.mult), R=[os_t, rd_t], W=[y_t])
                    self.dma('sp', self.yaT[h * 64:(h + 1) * 64, g * 512:(g + 1) * 512], y_t[:, :], R=[y_t], owner=y_t)

                self.defer(2, epiA)
                self.defer(5, epiB)
            while self.bg_q:
                self.bg_q.pop(0)()
        self.flush_epi()
        self.end_phase()

    def phase3(self, l):
        nc = self.nc
        self.begin_phase()
        KR = 70
        Vc = self.tile("Vc", [128, NT, 512], BF16)
        self.dma('sp', Vc[:, 0:16, :], self.vc.rearrange("(kt p) c -> p kt c", p=128)[:, 0:16, :], W=[Vc], owner=Vc)
        self.dma('sp', Vc[:, 16:32, :], self.vc.rearrange("(kt p) c -> p kt c", p=128)[:, 16:32, :], W=[Vc], owner=Vc)
        Qc = [self.tile("Qc", [KR, T], BF16) for _ in range(3)]
        Kc = [self.tile("Kc", [KR, T], BF16) for _ in range(3)]
        pT = [self.tile("pT", [128, 512], BF16) for _ in range(4)]
        o1 = [self.tile("o1", [128, 512], F32) for _ in range(2)]
        o2 = [self.tile("o2", [128, 512], F32) for _ in range(2)]
        r1 = [self.tile("r1", [128, 512], F32) for _ in range(2)]
        r2 = [self.tile("r2", [128, 512], F32) for _ in range(2)]
        sq = [self.tile("sq", [128, 512], F32) for _ in range(2)]
        rin = [self.tile("rin", [128, 512], F32) for _ in range(2)]
        yst = [self.tile("yst", [128, 512], BF16) for _ in range(3)]
        pS = [self.psum("pS") for _ in range(3)]
        pO = [self.psum("pO") for _ in range(2)]
        pD = [self.psum("pD") for _ in range(2)]
        pX = self.psum("pX")
        self.n_s = 0
        self.n_e = 0
        self.epi_q = []
        self.bg_q = []
        self.n_tick = 0
        self.call_id = 0
        n_o = 0
        n_qk = 0
        n_acc = 0
        for h in range(4):
            steep = len(self.attn_tiles(4, DIFF_SLOPES[h])) <= 10
            QK = []
            for m in range(2):
                Q_t, K_t = Qc[n_qk % 3], Kc[n_qk % 3]
                n_qk += 1
                r0 = h * 128 + m * 64
                self.dma('sp', Q_t[0:64, :], self.qcT[r0:r0 + 64, :], W=[Q_t], owner=Q_t)
                self.dma('pool', Q_t[64:70, :], self.qpos_d[:, :], W=[Q_t], owner=Q_t)
                self.dma('sp', K_t[0:64, :], self.kcT[r0:r0 + 64, :], W=[K_t], owner=K_t)
                self.dma('pool', K_t[64:70, :], self.kpos_d[8 + h], W=[K_t], owner=K_t)
                QK.append((Q_t, K_t))
            for g in range(NG):
                k = n_o % 2
                n_o += 1
                o1_t, o2_t, r1_t, r2_t, sq_t, ri_t = o1[k], o2[k], r1[k], r2[k], sq[k], rin[k]
                y_t = yst[n_o % 3]
                for m in range(2):
                    Q_t, K_t = QK[m]
                    pO_t, pD_t = pO[n_acc % 2], pD[n_acc % 2]
                    n_acc += 1

                    def pv(i, kt, c0, e_t, first, last, pO_t=pO_t, pD_t=pD_t):
                        self.op('pe', lambda: nc.tensor.matmul(pO_t[:, c0:512], Vc[:, kt, h * 128:(h + 1) * 128], e_t[:, c0:512], start=first, stop=last), R=[Vc, e_t], W=[pO_t])
                        self.op('pe', lambda: nc.tensor.matmul(pD_t[:, c0:512], self.ones_bf[:, 0:128], e_t[:, c0:512], start=first, stop=last), R=[self.ones_bf, e_t], W=[pD_t])

                    self.attn_group(K_t, Q_t, KR, g, DIFF_SLOPES[h], pS, pT, pv)

                    def epiA(m=m, pO_t=pO_t, pD_t=pD_t, o1_t=o1_t, o2_t=o2_t, r1_t=r1_t, r2_t=r2_t, sq_t=sq_t, steep=steep):
                        od, rd = (o1_t, r1_t) if m == 0 else (o2_t, r2_t)
                        if steep:
                            self.op('act', lambda: nc.scalar.copy(od[:, :], pO_t[:, :]), R=[pO_t], W=[od])
                            self.op('act', lambda: nc.scalar.activation(out=rd[:, :], in_=pD_t[:, :], func=AF.Ln), R=[pD_t], W=[rd])
                            self.op('act', lambda: nc.scalar.activation(out=rd[:, :], in_=rd[:, :], func=AF.Exp, scale=-1.0), R=[rd], W=[rd])
                        else:
                            self.op('dve', lambda: nc.vector.tensor_copy(od[:, :], pO_t[:, :]), R=[pO_t], W=[od])
                            self.op('dve', lambda: nc.vector.reciprocal(rd[:, :], pD_t[:, :]), R=[pD_t], W=[rd])
                        self.op('dve', lambda: nc.vector.tensor_tensor(od[:], od[:], rd[:], ALU.mult), R=[od, rd], W=[od])
                        if m == 0:
                            return
                        self.op('dve', lambda: nc.vector.scalar_tensor_tensor(o1_t[:], o2_t[:], self.nlam[:, l:l + 1], o1_t[:], ALU.mult, ALU.add), R=[o1_t, o2_t, self.nlam], W=[o1_t])
                        self.op('pool', lambda: nc.gpsimd.tensor_tensor(sq_t[:], o1_t[:], o1_t[:], ALU.mult), R=[o1_t], W=[sq_t])

                    def epiC(o1_t=o1_t, sq_t=sq_t, ri_t=ri_t, y_t=y_t, h=h, g=g):
                        self.op('pe', lambda: nc.tensor.matmul(pX[:, :], self.ones_f[:, :], sq_t[:, :], start=True, stop=True), R=[self.ones_f, sq_t], W=[pX])
                        self.op('act', lambda: nc.scalar.activation(out=ri_t[:], in_=pX[:, :], func=AF.Ln, scale=1.0 / 128, bias=self.eps_t[:, 0:1]), R=[pX, self.eps_t], W=[ri_t])
                        self.op('act', lambda: nc.scalar.activation(out=ri_t[:], in_=ri_t[:], func=AF.Exp, scale=-0.5), R=[ri_t], W=[ri_t])
                        self.op('dve', lambda: nc.vector.scalar_tensor_tensor(y_t[:], o1_t[:], self.gsub[:, l:l + 1], ri_t[:], ALU.mult, ALU.mult), R=[o1_t, self.gsub, ri_t], W=[y_t])
                        self.dma('sp', self.ycT[h * 128:(h + 1) * 128, g * 512:(g + 1) * 512], y_t[:], R=[y_t], owner=y_t)

                    self.defer(2, epiA)
                    if m == 1:
                        self.defer(8, epiC)
        self.flush_epi()
        self.end_phase()

    def phase5(self, l):
        nc = self.nc
        self.begin_phase()
        xsrc = self.x_in if l == 0 else self.xA
        WA = self.tile("WA", [128, 4, D], BF16)
        WB = self.tile("WB", [128, 8, D], BF16)
        WC = self.tile("WC", [128, 4, D], BF16)
        WO = self.tile("WO", [128, 8, D], BF16)
        self.dma('pool', WA[:], self.w_a[l].rearrange("(kc p) n -> p kc n", p=128), W=[WA], owner=WA)
        self.dma('pool', WB[:], self.w_b[l].rearrange("(kc p) n -> p kc n", p=128), W=[WB], owner=WB)
        self.dma('pool', WC[:], self.w_c[l].rearrange("(kc p) n -> p kc n", p=128), W=[WC], owner=WC)
        self.dma('pool', WO[:], self.w_o[l].rearrange("(kc p) n -> p kc n", p=128), W=[WO], owner=WO)
        ya = [self.tile("ya", [128, 4, 512], BF16) for _ in range(2)]
        yb = [self.tile("yb", [128, 8, 512], BF16) for _ in range(2)]
        yc = [self.tile("yc", [128, 4, 512], BF16) for _ in range(2)]
        gt = [self.tile("gt", [128, 24, 512], BF16) for _ in range(2)]
        mg = [self.tile("mg", [128, 8, 512], BF16) for _ in range(2)]
        t1 = [self.tile("t1", [128, 512], F32) for _ in range(2)]
        t2 = [self.tile("t2", [128, 512], F32) for _ in range(2)]
        xp = [self.tile("xp", [128, D], F32) for _ in range(3)]
        xo = [self.tile("xo", [128, D], F32) for _ in range(3)]
        pA = [self.psum("pA") for _ in range(2)]
        pBk = [self.psum("pBk") for _ in range(2)]
        pC = [self.psum("pC") for _ in range(2)]
        pOut = [self.psum("pOut") for _ in range(2)]
        n = 0
        nx = 0
        npo = 0
        for g in range(NG):
            k = g % 2
            ya_t, yb_t, yc_t, gt_t, mg_t = ya[k], yb[k], yc[k], gt[k], mg[k]
            sl = slice(g * 512, (g + 1) * 512)
            self.dma('sp', ya_t[:], self.yaT.rearrange("(c p) t -> p c t", p=128)[:, :, sl], W=[ya_t], owner=ya_t)
            self.dma('sp', yb_t[:], self.ybT.rearrange("(c p) t -> p c t", p=128)[:, :, sl], W=[yb_t], owner=yb_t)
            self.dma('sp', yc_t[:], self.ycT.rearrange("(c p) t -> p c t", p=128)[:, :, sl], W=[yc_t], owner=yc_t)
            self.dma('sp', gt_t[:, 0:12, :], self.gT.rearrange("(c p) t -> p c t", p=128)[:, 0:12, sl], W=[gt_t], owner=gt_t)
            self.dma('sp', gt_t[:, 12:24, :], self.gT.rearrange("(c p) t -> p c t", p=128)[:, 12:24, sl], W=[gt_t], owner=gt_t)
            for oc in range(8):
                kk = n % 2
                n += 1
                a_p, b_p, c_p = pA[kk], pBk[kk], pC[kk]
                cs = slice(oc * 128, (oc + 1) * 128)
                for kc in range(4):
                    self.op('pe', lambda: nc.tensor.matmul(a_p[:, :], WA[:, kc, cs], ya_t[:, kc, :], start=(kc == 0), stop=(kc == 3)), R=[WA, ya_t], W=[a_p])
                for kc in range(8):
                    self.op('pe', lambda: nc.tensor.matmul(b_p[:, :], WB[:, kc, cs], yb_t[:, kc, :], start=(kc == 0), stop=(kc == 7)), R=[WB, yb_t], W=[b_p])
                for kc in range(4):
                    self.op('pe', lambda: nc.tensor.matmul(c_p[:, :], WC[:, kc, cs], yc_t[:, kc, :], start=(kc == 0), stop=(kc == 3)), R=[WC, yc_t], W=[c_p])
                t1_t, t2_t = t1[kk], t2[kk]
                self.op('dve', lambda: nc.vector.tensor_tensor(t1_t[:], a_p[:, :], gt_t[:, oc, :], ALU.mult), R=[a_p, gt_t], W=[t1_t])
                self.op('dve', lambda: nc.vector.tensor_tensor(t2_t[:], b_p[:, :], gt_t[:, 8 + oc, :], ALU.mult), R=[b_p, gt_t], W=[t2_t])
                self.op('pool', lambda: nc.gpsimd.tensor_tensor(t1_t[:], t1_t[:], t2_t[:], ALU.add), R=[t1_t, t2_t], W=[t1_t])
                self.op('dve', lambda: nc.vector.tensor_tensor(t2_t[:], c_p[:, :], gt_t[:, 16 + oc, :], ALU.mult), R=[c_p, gt_t], W=[t2_t])
                self.op('pool', lambda: nc.gpsimd.tensor_tensor(mg_t[:, oc, :], t1_t[:], t2_t[:], ALU.add), R=[t1_t, t2_t], W=[mg_t])
            for s in range(4):
                x_t = xp[nx % 3]
                xo_t = xo[nx % 3]
                nx += 1
                r0 = g * 512 + s * 128
                self.dma('sp', x_t[:], xsrc[r0:r0 + 128, :], W=[x_t], owner=x_t)
                for half in range(2):
                    o_p = pOut[npo % 2]
                    npo += 1
                    hs_ = slice(half * 512, (half + 1) * 512)
                    for kc in range(8):
                        self.op('pe', lambda: nc.tensor.matmul(o_p[:, :], mg_t[:, kc, s * 128:(s + 1) * 128], WO[:, kc, hs_], start=(kc == 0), stop=(kc == 7)), R=[mg_t, WO], W=[o_p])
                    self.op('dve', lambda: nc.vector.tensor_tensor(xo_t[:, hs_], o_p[:, :], x_t[:, hs_], ALU.add), R=[o_p, x_t], W=[xo_t])
                self.dma('sp', self.xB[r0:r0 + 128, :], xo_t[:], R=[xo_t], owner=xo_t)
        self.end_phase()

    def phase6(self, l):
        nc = self.nc
        pb = l * NPV
        self.begin_phase()
        hT = self.tile("h2T", [128, 8, T], BF16)
        self.norm_transpose(self.xB, pb + PV_GFFN, hT)
        wv = self.f_up[l].rearrange("(kc p) n -> p kc n", p=128)
        wu = [self.tile("wu", [128, 8, 256], BF16) for _ in range(3)]
        cc = [self.tile("cc", [128, 2, 512], F32) for _ in range(3)]
        s1 = [self.tile("s1", [128, 2, 520], F32) for _ in range(2)]
        s0 = [self.tile("s0", [128, 2, 520], F32) for _ in range(2)]
        ast = [self.tile("ast", [128, 512], BF16) for _ in range(3)]
        ps = [self.psum("p6ps") for _ in range(6)]

        def load_u(j):
            w_t = wu[j % 3]
            self.dma('pool', w_t[:, :, 0:128], wv[:, :, j * 128:(j + 1) * 128], W=[w_t], owner=w_t)
            self.dma('pool', w_t[:, :, 128:256], wv[:, :, DFF + j * 128:DFF + (j + 1) * 128], W=[w_t], owner=w_t)

        def stageA(it, j, g):
            w_t = wu[j % 3]
            cwg = lambda i: self.pvec[:, pb + PV_FCW + i * 44 + j:pb + PV_FCW + i * 44 + j + 1]
            cwv = lambda i: self.pvec[:, pb + PV_FCW + i * 44 + 22 + j:pb + PV_FCW + i * 44 + 22 + j + 1]
            cbg = self.pvec[:, pb + PV_FCB + j:pb + PV_FCB + j + 1]
            cbv = self.pvec[:, pb + PV_FCB + 22 + j:pb + PV_FCB + 22 + j + 1]
            k = it % 2
            p_g, p_v = ps[(2 * it) % 6], ps[(2 * it + 1) % 6]
            for kc in range(8):
                self.op('pe', lambda: nc.tensor.matmul(p_g[:, :], w_t[:, kc, 0:128], hT[:, kc, g * 512:(g + 1) * 512], start=(kc == 0), stop=(kc == 7)), R=[w_t, hT], W=[p_g])
            for kc in range(8):
                self.op('pe', lambda: nc.tensor.matmul(p_v[:, :], w_t[:, kc, 128:256], hT[:, kc, g * 512:(g + 1) * 512], start=(kc == 0), stop=(kc == 7)), R=[w_t, hT], W=[p_v])
            c_t, s1_t, s0_t = cc[it % 3], s1[k], s0[k]
            for (cur, prv, n) in ((s1_t, s1[1 - k], 1), (s0_t, s0[1 - k], 2)):
                if g == 0:
                    self.op('dve', lambda: nc.vector.memset(cur[:, :, 4:4 + n], 0.0), W=[cur])
                else:
                    self.op('dve', lambda: nc.vector.tensor_copy(cur[:, :, 4:4 + n], prv[:, :, 516:516 + n]), R=[prv], W=[cur])
            self.op('act', lambda: nc.scalar.activation(out=c_t[:, 0, :], in_=p_g[:, :], func=AF.Identity, scale=cwg(2), bias=cbg), R=[p_g, self.pvec], W=[c_t])
            self.op('act', lambda: nc.scalar.activation(out=s1_t[:, 0, 5:517], in_=p_g[:, :], func=AF.Identity, scale=cwg(1), bias=self.zero_t[:, 0:1]), R=[p_g, self.pvec], W=[s1_t])
            self.op('act', lambda: nc.scalar.activation(out=s0_t[:, 0, 6:518], in_=p_g[:, :], func=AF.Identity, scale=cwg(0), bias=self.zero_t[:, 0:1]), R=[p_g, self.pvec], W=[s0_t])
            self.op('dve', lambda: nc.vector.tensor_scalar_mul(s0_t[:, 1, 6:518], p_v[:, :], cwv(0)), R=[p_v, self.pvec], W=[s0_t])
            self.op('act', lambda: nc.scalar.activation(out=c_t[:, 1, :], in_=p_v[:, :], func=AF.Identity, scale=cwv(2), bias=cbv), R=[p_v, self.pvec], W=[c_t])
            self.op('act', lambda: nc.scalar.activation(out=s1_t[:, 1, 5:517], in_=p_v[:, :], func=AF.Identity, scale=cwv(1), bias=self.zero_t[:, 0:1]), R=[p_v, self.pvec], W=[s1_t])
            self.op('dve', lambda: nc.vector.tensor_tensor(c_t[:, :, :], c_t[:, :, :], s1_t[:, :, 4:516], ALU.add), R=[c_t, s1_t], W=[c_t])
            self.op('dve', lambda: nc.vector.tensor_tensor(c_t[:, 0, :], c_t[:, 0, :], s0_t[:, 0, 4:516], ALU.add), R=[c_t, s0_t], W=[c_t])
            self.op('pool', lambda: nc.gpsimd.tensor_tensor(c_t[:, 1, :], c_t[:, 1, :], s0_t[:, 1, 4:516], ALU.add), R=[c_t, s0_t], W=[c_t])

        def stageB(it, j, g):
            c_t = cc[it % 3]
            self.op('act', lambda: nc.scalar.activation(out=c_t[:, 0, :], in_=c_t[:, 0, :], func=AF.Gelu_apprx_tanh), R=[c_t], W=[c_t])
            a_t = ast[it % 3]
            self.op('pool', lambda: nc.gpsimd.tensor_tensor(a_t[:], c_t[:, 0, :], c_t[:, 1, :], ALU.mult), R=[c_t], W=[a_t])
            self.dma('sp', self.aT[j * 128:(j + 1) * 128, g * 512:(g + 1) * 512], a_t[:], R=[a_t], owner=a_t)

        load_u(0)
        prev = None
        it = 0
        for j in range(NFC):
            if j + 1 < NFC:
                load_u(j + 1)
            for g in range(NG):
                stageA(it, j, g)
                if prev is not None:
                    stageB(*prev)
                prev = (it, j, g)
                it += 1
        stageB(*prev)
        self.end_phase()
        self.begin_phase()
        WD = self.tile("WD", [128, NFC, D], BF16)
        dv = self.f_dn[l].rearrange("(kc p) n -> p kc n", p=128)
        self.dma('pool', WD[:, 0:11, :], dv[:, 0:11, :], W=[WD], owner=WD)
        self.dma('pool', WD[:, 11:22, :], dv[:, 11:22, :], W=[WD], owner=WD)
        at = [self.tile("at", [128, NFC, 512], BF16) for _ in range(2)]
        xp = [self.tile("xp", [128, D], F32) for _ in range(3)]
        xo = [self.tile("xo", [128, D], F32) for _ in range(3)]
        pOut = [self.psum("pOut") for _ in range(4)]
        nx = 0
        npo = 0
        for g in range(NG):
            a_t = at[g % 2]
            sl = slice(g * 512, (g + 1) * 512)
            av = self.aT.rearrange("(c p) t -> p c t", p=128)
            self.dma('sp', a_t[:, 0:11, :], av[:, 0:11, sl], W=[a_t], owner=a_t)
            self.dma('sp', a_t[:, 11:22, :], av[:, 11:22, sl], W=[a_t], owner=a_t)
            for s in range(4):
                x_t = xp[nx % 3]
                xo_t = xo[nx % 3]
                nx += 1
                r0 = g * 512 + s * 128
                self.dma('sp', x_t[:], self.xB[r0:r0 + 128, :], W=[x_t], owner=x_t)
                for half in range(2):
                    o_p = pOut[npo % 4]
                    npo += 1
                    hs_ = slice(half * 512, (half + 1) * 512)
                    for kc in range(NFC):
                        self.op('pe', lambda: nc.tensor.matmul(o_p[:, :], a_t[:, kc, s * 128:(s + 1) * 128], WD[:, kc, hs_], start=(kc == 0), stop=(kc == NFC - 1)), R=[a_t, WD], W=[o_p])
                    self.op('dve', lambda: nc.vector.tensor_tensor(xo_t[:, hs_], o_p[:, :], x_t[:, hs_], ALU.add), R=[o_p, x_t], W=[xo_t])
                self.dma('sp', self.xA[r0:r0 + 128, :], xo_t[:], R=[xo_t], owner=xo_t)
        self.end_phase()

    def final(self, src):
        nc = self.nc
        self.begin_phase()
        gf = self.tile("gf", [128, D], F32)
        self.dma('sp', gf[:], self.gfin_d[:, :], W=[gf], owner=gf)
        xp = [self.tile("fx", [128, D], F32) for _ in range(3)]
        xo = [self.tile("fo", [128, D], F32) for _ in range(3)]
        junk = self.tile("fjunk", [128, D], BF16)
        ss = [self.tile("fss", [128, 1], F32) for _ in range(3)]
        rs = [self.tile("frs", [128, 1], F32) for _ in range(3)]
        for tt in range(NT):
            x_t, o_t, ss_t, rs_t = xp[tt % 3], xo[tt % 3], ss[tt % 3], rs[tt % 3]
            self.dma('sp', x_t[:], src[tt * 128:(tt + 1) * 128, :], W=[x_t], owner=x_t)
            self.op('dve', lambda: nc.vector.memset(ss_t[:], 0.0), W=[ss_t])
            self.op('act', lambda: nc.scalar.activation(out=junk[:], in_=x_t[:], func=AF.Square, accum_out=ss_t[:]), R=[x_t, ss_t], W=[junk, ss_t])
            self.op('act', lambda: nc.scalar.activation(out=rs_t[:], in_=ss_t[:], func=AF.Sqrt, scale=1.0 / D, bias=self.eps_t[:, 0:1]), R=[ss_t, self.eps_t], W=[rs_t])
            self.op('dve', lambda: nc.vector.reciprocal(rs_t[:], rs_t[:]), R=[rs_t], W=[rs_t])
            self.op('dve', lambda: nc.vector.scalar_tensor_tensor(o_t[:], x_t[:], rs_t[:, 0:1], gf[:], ALU.mult, ALU.mult), R=[x_t, rs_t, gf], W=[o_t])
            self.dma('sp', self.out[tt * 128:(tt + 1) * 128, :], o_t[:], R=[o_t], owner=o_t)
        self.end_phase()

    def build(self):
        self.declare()
        self.setup()
        stop = self.stop_after
        done = False
        for l in range(self.NL):
            for name, fn in (("p1", self.phase1), ("p2", self.phase2), ("p3", self.phase3), ("p5", self.phase5), ("p6", self.phase6)):
                fn(l)
                if stop == (l, name):
                    done = True
                    break
            if done:
                break
        if not done:
            self.final(self.xA)
        self.top.close()
        return self.nc


def _host_consts():
    c = {}
    ident = np.eye(128, dtype=np.float32)
    kk = np.arange(128)[:, None]
    qq = np.arange(128)[None, :]
    tri = np.where(qq >= kk, 0.0, -BIG).astype(np.float32)
    c["cbf"] = np.concatenate([ident, tri], axis=1)
    pos = np.arange(T)
    pa, pbb, pc = pos // 256, (pos // 16) % 16, pos % 16
    c["qpos"] = np.stack([pa, pbb, pc, np.ones(T), np.ones(T), np.ones(T)]).astype(np.float32)
    slopes = [2.0 ** (-8.0 * (h + 1) / 8) for h in range(8)] + [2.0 ** (-8.0 * (h + 1) / 4) for h in range(4)]
    kp = np.zeros((12, 6, T), np.float32)
    for i, s in enumerate(slopes):
        kp[i, 0] = -8 * s * 256
        kp[i, 1] = -8 * s * 16
        kp[i, 2] = -8 * s
        kp[i, 3] = 8 * s * 256 * pa
        kp[i, 4] = 8 * s * 16 * pbb
        kp[i, 5] = 8 * s * pc
    c["kpos"] = kp
    c["koh"] = (np.arange(16)[:, None] == (pos // 256)[None, :]).astype(np.float32)
    qt = np.arange(32)
    jb = qt // 2
    n = np.arange(16)
    pastneg = np.where(n[None, :] < jb[:, None], 0.0, -1e30).astype(np.float32)
    Cq = np.where(n[None, :] == jb[:, None], 0.0, -BIG).astype(np.float32)
    gt = np.concatenate([pastneg.reshape(-1), Cq.reshape(-1)])
    c["gtab"] = np.ascontiguousarray(np.broadcast_to(gt[None, :], (128, gt.size))).astype(np.float32)
    return c


def _fm(v, nch):
    return np.asarray(v, np.float32).reshape(nch, 128).T


def _host_layout(inp):
    m = {}
    pv = np.zeros((128, 4, NPV), np.float32)
    bd = np.zeros((4, 2, 8, 128, 128), np.float32)
    lqk = np.zeros((1, 4, 4, 64), np.float32)
    for l in range(4):
        pv[:, l, PV_GMIX:PV_GMIX + 8] = _fm(inp["norm_mix_g"][l], 8)
        pv[:, l, PV_GFFN:PV_GFFN + 8] = _fm(inp["norm_ffn_g"][l], 8)
        pv[:, l, PV_BGATE:PV_BGATE + 24] = _fm(inp["b_gate"][l], 24)
        for i in range(4):
            pv[:, l, PV_RCW + i * 8:PV_RCW + i * 8 + 8] = _fm(inp["rg_conv_w"][l, i], 8)
        pv[:, l, PV_RCB:PV_RCB + 8] = _fm(inp["rg_conv_b"][l], 8)
        pv[:, l, PV_RBA:PV_RBA + 8] = _fm(inp["rg_b_a"][l], 8)
        pv[:, l, PV_RBX:PV_RBX + 8] = _fm(inp["rg_b_x"][l], 8)
        pv[:, l, PV_RLAM:PV_RLAM + 8] = _fm(inp["rg_lambda"][l], 8)
        pv[:, l, PV_SUBG] = np.asarray(inp["diff_subln_g"][l], np.float32)
        for i in range(3):
            pv[:, l, PV_FCW + i * 44:PV_FCW + i * 44 + 44] = _fm(inp["ffn_conv_w"][l, i], 44)
        pv[:, l, PV_FCB:PV_FCB + 44] = _fm(inp["ffn_conv_b"][l], 44)
        for w, key in enumerate(("rg_w_a", "rg_w_x")):
            for j in range(8):
                for q in range(2):
                    bd[l, w, j, q * 64:(q + 1) * 64, q * 64:(q + 1) * 64] = inp[key][l, 2 * j + q]
        for w, key in enumerate(("diff_lq1", "diff_lk1", "diff_lq2", "diff_lk2")):
            lqk[0, l, w] = inp[key][l]
    m["pvec"] = pv.reshape(128, 4 * NPV)
    m["bdiag"] = bd
    m["lqk"] = np.ascontiguousarray(np.broadcast_to(lqk.reshape(1, -1), (128, 1024)))
    m["gfin"] = np.ascontiguousarray(np.broadcast_to(np.asarray(inp["final_norm_g"], np.float32)[None, :], (128, D)))
    for k in ("w_in", "w_branch_a", "w_branch_b", "w_branch_c", "w_out", "ffn_up", "ffn_down"):
        m[k] = np.ascontiguousarray(np.asarray(inp[k], np.float32))
    m.update(_host_consts())
    return m


def kernel(**inputs):
    x = np.asarray(inputs["x"], np.float32)
    shared = _host_layout(inputs)
    kb = KB(NL=4)
    nc = kb.build()
    in_maps = []
    for c in range(8):
        mm = dict(shared)
        mm["x"] = np.ascontiguousarray(x[c % 4])
        in_maps.append(mm)
    res = run_bass_kernel_spmd(nc, in_maps, core_ids=list(range(8)))
    out = np.stack([np.asarray(res.results[b]["out"], np.float32) for b in range(4)], axis=0)
    return out
```

```python
import contextlib
import numpy as np
import ml_dtypes
import concourse.bass as bass
import concourse.mybir as mybir
from concourse.bass_utils import run_bass_kernel_spmd

F32 = mybir.dt.float32
BF16 = mybir.dt.bfloat16
AF = mybir.ActivationFunctionType
ALU = mybir.AluOpType
AX = mybir.AxisListType

T = 4096
D = 1024
NG = 8
NT = 32
DFF = 2816
NFC = 22
EPS = 1e-6
BIG = 32768.0
ALIBI_CUT = 48.0
MOBA_SLOPES = [2.0 ** (-(h + 1)) for h in range(8)]
DIFF_SLOPES = [2.0 ** (-2.0 * (h + 1)) for h in range(4)]
NPV = 281
O_QA, O_KA, O_VA, O_QC, O_KC, O_VC, O_RX, O_RG, O_GT = 0, 512, 1024, 1536, 2048, 2560, 3072, 4096, 5120
PV_GMIX, PV_GFFN, PV_BGATE, PV_RCW, PV_RCB, PV_RBA, PV_RBX, PV_RLAM, PV_SUBG, PV_FCW, PV_FCB = \
    0, 8, 16, 40, 72, 80, 88, 96, 104, 105, 237


class Tile:
    def __init__(self, kb, t, name):
        self.kb = kb
        self.t = t
        self.name = name
        self.w = None
        self.r = []
        self.dsem = None
        self.dcnt = 0
        self.is_psum = False

    def __getitem__(self, idx):
        return self.t[idx]


class KB:
    def __init__(self, NL=4, debug=False, stop_after=None):
        self.NL = NL
        self.debug = debug
        self.stop_after = stop_after
        nc = self.nc = bass.Bass("TRN2", target_bir_lowering=False)
        self.E = {'pe': nc.tensor, 'act': nc.scalar, 'dve': nc.vector, 'pool': nc.gpsimd, 'sp': nc.sync}
        self.cnt = {e: 0 for e in self.E}
        self.seen = {e: {} for e in self.E}
        self.top = contextlib.ExitStack()
        self.sem = {e: self.top.enter_context(nc.semaphore("s_" + e)) for e in self.E}
        self.bar_sem = self.top.enter_context(nc.semaphore("s_bar"))
        self.nbar = 0
        self.dsem_free = [self.top.enter_context(nc.semaphore("s_d%d" % i)) for i in range(64)]
        self.dsem_cnt = {}
        self.phase = None
        self.phase_tiles = []
        self.persist_tiles = []
        self.uid = 0

    def _mk(self, es, lst, name, shape, dt, psum=False):
        self.uid += 1
        nm = "%s_%d" % (name, self.uid)
        if psum:
            t = es.enter_context(self.nc.psum_tensor(nm, shape, dt))
        else:
            t = es.enter_context(self.nc.sbuf_tensor(nm, shape, dt))
        tl = Tile(self, t, nm)
        tl.is_psum = psum
        lst.append(tl)
        return tl

    def ptile(self, name, shape, dt):
        return self._mk(self.top, self.persist_tiles, name, shape, dt)

    def tile(self, name, shape, dt):
        return self._mk(self.phase, self.phase_tiles, name, shape, dt)

    def psum(self, name, shape=None, dt=F32):
        return self._mk(self.phase, self.phase_tiles, name, shape or [128, 512], dt, psum=True)

    def begin_phase(self):
        self.phase = contextlib.ExitStack()
        self.phase_tiles = []

    def end_phase(self):
        self.barrier()
        for tl in self.phase_tiles:
            if tl.dsem is not None:
                self.dsem_free.append(tl.dsem)
                tl.dsem = None
        self.phase.close()
        self.phase = None
        self.phase_tiles = []

    def _wait(self, e, evs):
        eng = self.E[e]
        need = {}
        for (sem, val, owner) in evs:
            if owner is not None and owner.dsem is sem:
                val = max(val, owner.dcnt)
            if val > need.get(sem, (0, None))[0]:
                need[sem] = (val, sem)
        for sem, (val, _) in need.items():
            if self.seen[e].get(sem, 0) >= val:
                continue
            if sem is self.sem[e] and (e == 'pe' or val <= self.cnt[e] - 4):
                continue
            eng.wait_ge(sem, val)
            self.seen[e][sem] = val

    @staticmethod
    def _gather(R, W):
        evs = []
        for r in R:
            if r.w is not None:
                evs.append(r.w)
            if r.is_psum:
                evs.extend(r.r)
        for w in W:
            if w.w is not None:
                evs.append(w.w)
            evs.extend(w.r)
        return evs

    @staticmethod
    def _record(ev, R, W):
        for r in R:
            r.r = [x for x in r.r if x[0] is not ev[0]] + [ev]
        for w in W:
            w.w = ev
            w.r = []

    def op(self, e, fn, R=(), W=()):
        self._wait(e, self._gather(R, W))
        inst = fn()
        self.cnt[e] += 1
        inst.then_inc(self.sem[e], 1)
        ev = (self.sem[e], self.cnt[e], None)
        self._record(ev, R, W)
        return inst

    def dma(self, q, out, in_, R=(), W=(), owner=None):
        self._wait(q, self._gather(R, W))
        if owner.dsem is None:
            owner.dsem = self.dsem_free.pop()
            owner.dcnt = self.dsem_cnt.get(owner.dsem, 0)
        inst = self.E[q].dma_start(out=out, in_=in_)
        owner.dcnt += 16
        self.dsem_cnt[owner.dsem] = owner.dcnt
        inst.then_inc(owner.dsem, 16)
        ev = (owner.dsem, owner.dcnt, owner)
        self._record(ev, R, W)
        return inst

    def barrier(self):
        sp = self.E['sp']
        for e in self.E:
            if e == 'sp' or self.cnt[e] == 0:
                continue
            if self.seen['sp'].get(self.sem[e], 0) < self.cnt[e]:
                sp.wait_ge(self.sem[e], self.cnt[e])
                self.seen['sp'][self.sem[e]] = self.cnt[e]
        for sem, c in self.dsem_cnt.items():
            if self.seen['sp'].get(sem, 0) < c:
                sp.wait_ge(sem, c)
                self.seen['sp'][sem] = c
        self.nbar += 1
        sp.sem_inc(self.bar_sem, 1)
        for e in self.E:
            if e == 'sp':
                continue
            self.E[e].wait_ge(self.bar_sem, self.nbar)
            for e2 in self.E:
                self.seen[e][self.sem[e2]] = self.cnt[e2]
            for sem, c in self.dsem_cnt.items():
                self.seen[e][sem] = c
        for e2 in self.E:
            self.seen['sp'][self.sem[e2]] = self.cnt[e2]
        for tl in self.phase_tiles + self.persist_tiles:
            tl.w = None
            tl.r = []

    def dram(self, name, shape, dt, kind="Internal"):
        if self.debug and kind == "Internal":
            kind = "ExternalOutput"
        return self.nc.dram_tensor(name, shape, dt, kind=kind).ap()

    def declare(self):
        NL = self.NL
        d = self.dram
        ei = "ExternalInput"
        self.x_in = d("x", [T, D], F32, ei)
        self.w_in = d("w_in", [4, D, 8192], F32, ei)
        self.w_a = d("w_branch_a", [4, 512, D], F32, ei)
        self.w_b = d("w_branch_b", [4, D, D], F32, ei)
        self.w_c = d("w_branch_c", [4, 512, D], F32, ei)
        self.w_o = d("w_out", [4, D, D], F32, ei)
        self.f_up = d("ffn_up", [4, D, 2 * DFF], F32, ei)
        self.f_dn = d("ffn_down", [4, DFF, D], F32, ei)
        self.pvec_d = d("pvec", [128, 4 * NPV], F32, ei)
        self.bd_d = d("bdiag", [4, 2, 8, 128, 128], F32, ei)
        self.lqk_d = d("lqk", [128, 4 * 4 * 64], F32, ei)
        self.gfin_d = d("gfin", [128, D], F32, ei)
        self.cbf_d = d("cbf", [128, 256], F32, ei)
        self.qpos_d = d("qpos", [6, T], F32, ei)
        self.kpos_d = d("kpos", [12, 6, T], F32, ei)
        self.koh_d = d("koh", [16, T], F32, ei)
        self.gtab_d = d("gtab", [128, 2 * 32 * 16], F32, ei)
        self.out = d("out", [T, D], F32, "ExternalOutput")
        self.xA = d("xA", [T, D], F32)
        self.xB = d("xB", [T, D], F32)
        self.qaT = d("qaT", [512, T], BF16)
        self.kaT = d("kaT", [512, T], BF16)
        self.va = d("va", [T, 512], BF16)
        self.qcT = d("qcT", [512, T], BF16)
        self.kcT = d("kcT", [512, T], BF16)
        self.vc = d("vc", [T, 512], BF16)
        self.gT = d("gT", [3072, T], BF16)
        self.yaT = d("yaT", [512, T], BF16)
        self.ybT = d("ybT", [D, T], BF16)
        self.ycT = d("ycT", [512, T], BF16)
        self.aT = d("aT", [DFF, T], BF16)

    def setup(self):
        nc = self.nc
        NL = self.NL
        self.pvec = self.ptile("pvec", [128, 4 * NPV], F32)
        self.cbf = self.ptile("cbf", [128, 256], BF16)
        self.ones_bf = self.ptile("ones_bf", [128, 128], BF16)
        self.ones_f = self.ptile("ones_f", [128, 128], F32)
        self.eps_t = self.ptile("eps", [128, 1], F32)
        self.one_t = self.ptile("one", [128, 1], F32)
        self.zero_t = self.ptile("zero", [128, 1], F32)
        self.sA = self.ptile("sA", [128, 4 * 8], F32)
        self.sA2 = self.ptile("sA2", [128, 4 * 8], F32)
        self.gsub = self.ptile("gsub", [128, 4], F32)
        self.nlam = self.ptile("nlam", [128, 4], F32)
        self.begin_phase()
        self.dma('sp', self.pvec[:], self.pvec_d[:, :], W=[self.pvec], owner=self.pvec)
        self.dma('pool', self.cbf[:], self.cbf_d[:, :], W=[self.cbf], owner=self.cbf)
        self.ident = self.cbf
        self.op('dve', lambda: nc.vector.memset(self.ones_bf[:], 1.0), W=[self.ones_bf])
        self.op('dve', lambda: nc.vector.memset(self.ones_f[:], 1.0), W=[self.ones_f])
        self.op('dve', lambda: nc.vector.memset(self.eps_t[:], EPS), W=[self.eps_t])
        self.op('dve', lambda: nc.vector.memset(self.one_t[:], 1.0), W=[self.one_t])
        self.op('dve', lambda: nc.vector.memset(self.zero_t[:], 0.0), W=[self.zero_t])
        tmp = self.tile("tmp_sa", [128, 4 * 8], F32)
        pv4 = self.pvec[:, :].rearrange("p (l c) -> p l c", l=4)
        lam_v = pv4[:, :, PV_RLAM:PV_RLAM + 8]
        tmp_v = tmp[:, :].rearrange("p (l c) -> p l c", l=4)
        self.op('act', lambda: nc.scalar.activation(out=tmp_v, in_=lam_v, func=AF.Exp, scale=-1.0), R=[self.pvec], W=[tmp])
        self.op('act', lambda: nc.scalar.activation(out=tmp[:], in_=tmp[:], func=AF.Ln, bias=self.one_t[:, 0:1], scale=1.0), R=[tmp, self.one_t], W=[tmp])
        self.op('dve', lambda: nc.vector.tensor_scalar_mul(self.sA[:], tmp[:], -8.0), R=[tmp], W=[self.sA])
        self.op('dve', lambda: nc.vector.tensor_scalar_mul(self.sA2[:], tmp[:], -16.0), R=[tmp], W=[self.sA2])
        import math
        self.lam_init = [0.8 - 0.6 * math.exp(-0.3 * l) for l in range(4)]
        for l in range(4):
            self.op('dve', lambda: nc.vector.tensor_scalar_mul(self.gsub[:, l:l + 1], self.pvec[:, l * NPV + PV_SUBG:l * NPV + PV_SUBG + 1], 1.0 - self.lam_init[l]), R=[self.pvec], W=[self.gsub])
        lqk = self.tile("lqk", [128, 4 * 4 * 64], F32)
        self.dma('sp', lqk[:], self.lqk_d[:, :], W=[lqk], owner=lqk)
        prod = self.tile("lprod", [128, 4 * 2 * 64], F32)
        lv = lqk[:, :].rearrange("p (l w d) -> p l w d", l=4, w=4)
        pvw = prod[:, :].rearrange("p (l w d) -> p l w d", l=4, w=2)
        self.op('dve', lambda: nc.vector.tensor_tensor(pvw[:, :, 0, :], lv[:, :, 0, :], lv[:, :, 1, :], ALU.mult), R=[lqk], W=[prod])
        self.op('dve', lambda: nc.vector.tensor_tensor(pvw[:, :, 1, :], lv[:, :, 2, :], lv[:, :, 3, :], ALU.mult), R=[lqk], W=[prod])
        ssum = self.tile("lsum", [128, 8], F32)
        self.op('dve', lambda: nc.vector.tensor_reduce(out=ssum[:, :], in_=prod[:, :].rearrange("p (a d) -> p a d", d=64), axis=AX.X, op=ALU.add), R=[prod], W=[ssum])
        self.op('act', lambda: nc.scalar.activation(out=ssum[:], in_=ssum[:], func=AF.Exp), R=[ssum], W=[ssum])
        sv = ssum[:, :].rearrange("p (l w) -> p l w", w=2)
        self.op('dve', lambda: nc.vector.tensor_tensor(self.nlam[:, :], sv[:, :, 1], sv[:, :, 0], ALU.subtract), R=[ssum], W=[self.nlam])
        for l in range(4):
            self.op('dve', lambda: nc.vector.tensor_scalar_add(self.nlam[:, l:l + 1], self.nlam[:, l:l + 1], -self.lam_init[l]), R=[self.nlam], W=[self.nlam])
        self.end_phase()

    def norm_transpose(self, xsrc, gcol0, hT):
        nc = self.nc
        xp = [self.tile("nx", [128, D], F32) for _ in range(3)]
        xn = [self.tile("nxn", [128, D], BF16) for _ in range(2)]
        junk = self.tile("njunk", [128, D], BF16)
        ss = [self.tile("nss", [128, 1], F32) for _ in range(3)]
        rs = [self.tile("nrs", [128, 1], F32) for _ in range(3)]
        pt = [self.psum("npt") for _ in range(2)]
        self.npt = pt
        for tt in range(NT):
            x_t = xp[tt % 3]
            xn_t = xn[tt % 2]
            ss_t = ss[tt % 3]
            rs_t = rs[tt % 3]
            self.dma('sp', x_t[:], xsrc[tt * 128:(tt + 1) * 128, :], W=[x_t], owner=x_t)
            self.op('dve', lambda: nc.vector.memset(ss_t[:], 0.0), W=[ss_t])
            self.op('act', lambda: nc.scalar.activation(out=junk[:], in_=x_t[:], func=AF.Square, accum_out=ss_t[:]), R=[x_t, ss_t], W=[junk, ss_t])
            self.op('act', lambda: nc.scalar.activation(out=rs_t[:], in_=ss_t[:], func=AF.Sqrt, scale=1.0 / D, bias=self.eps_t[:, 0:1]), R=[ss_t, self.eps_t], W=[rs_t])
            self.op('dve', lambda: nc.vector.reciprocal(rs_t[:], rs_t[:]), R=[rs_t], W=[rs_t])
            self.op('dve', lambda: nc.vector.tensor_scalar_mul(xn_t[:], x_t[:], rs_t[:, 0:1]), R=[x_t, rs_t], W=[xn_t])
            for half in range(2):
                p_t = pt[half]
                for c4 in range(4):
                    c = half * 4 + c4
                    self.op('pe', lambda: nc.tensor.matmul(p_t[:, c4 * 128:(c4 + 1) * 128], xn_t[:, c * 128:(c + 1) * 128], self.ident[:, 0:128], start=True, stop=True), R=[xn_t, self.ident], W=[p_t])
                gv = self.pvec[:, gcol0 + half * 4:gcol0 + half * 4 + 4].unsqueeze(2).broadcast_to([128, 4, 128])
                self.op('dve', lambda: nc.vector.tensor_tensor(hT[:, half * 4:half * 4 + 4, tt * 128:(tt + 1) * 128], p_t[:, :].rearrange("p (c t) -> p c t", c=4), gv, ALU.mult), R=[p_t, self.pvec], W=[hT])

    def phase1(self, l):
        nc = self.nc
        self.begin_phase()
        xsrc = self.x_in if l == 0 else self.xA
        pb = l * NPV
        hT = self.tile("hT", [128, 8, T], BF16)
        self.norm_transpose(xsrc, pb + PV_GMIX, hT)
        wv = self.w_in[l].rearrange("(kc p) n -> p kc n", p=128)
        wb = [self.tile("wb", [128, 8, 512], BF16) for _ in range(3)]
        st = [self.tile("st", [128, 4, 512], BF16) for _ in range(3)]
        ps = [self.psum("p1ps") for _ in range(4)]
        jobs = [(O_QA, self.qaT, 0, None, 'f'), (O_KA, self.kaT, 0, None, 'f'), (O_QC, self.qcT, 0, None, 'f'), (O_KC, self.kcT, 0, None, 'f')]
        for i in range(6):
            jobs.append((O_GT + i * 512, self.gT, i * 512, pb + PV_BGATE + i * 4, 'f'))
        jobs.append((O_VA, self.va, 0, None, 't'))
        jobs.append((O_VC, self.vc, 0, None, 't'))
        cnts = {'ps': 0, 'st': 0}

        def load_w(ji):
            c0 = jobs[ji][0]
            w_t = wb[ji % 3]
            self.dma('pool', w_t[:], wv[:, :, c0:c0 + 512], W=[w_t], owner=w_t)

        def units():
            load_w(0)
            for ji, (c0, dst, r0, bcol, kind) in enumerate(jobs):
                w_t = wb[ji % 3]
                if ji + 1 < len(jobs):
                    load_w(ji + 1)
                if kind == 'f':
                    dview = dst[r0:r0 + 512, :].rearrange("(m p) t -> p m t", p=128)
                    for g in range(NG):
                        s_t = st[cnts['st'] % 3]
                        cnts['st'] += 1
                        for m in range(4):
                            p_t = ps[cnts['ps'] % 4]
                            cnts['ps'] += 1
                            for kc in range(8):
                                self.op('pe', lambda: nc.tensor.matmul(p_t[:, :], w_t[:, kc, m * 128:(m + 1) * 128], hT[:, kc, g * 512:(g + 1) * 512], start=(kc == 0), stop=(kc == 7)), R=[w_t, hT], W=[p_t])
                            if bcol is not None:
                                self.op('act', lambda: nc.scalar.activation(out=s_t[:, m, :], in_=p_t[:, :], func=AF.Sigmoid, bias=self.pvec[:, bcol + m:bcol + m + 1], scale=1.0), R=[p_t, self.pvec], W=[s_t])
                            elif m % 2 == 0:
                                self.op('act', lambda: nc.scalar.copy(s_t[:, m, :], p_t[:, :]), R=[p_t], W=[s_t])
                            else:
                                self.op('dve', lambda: nc.vector.tensor_copy(s_t[:, m, :], p_t[:, :]), R=[p_t], W=[s_t])
                            if m == 3:
                                self.dma('sp', dview[:, :, g * 512:(g + 1) * 512], s_t[:], R=[s_t], owner=s_t)
                            yield
                else:
                    for tt in range(NT):
                        p_t = ps[cnts['ps'] % 4]
                        cnts['ps'] += 1
                        s_t = st[cnts['st'] % 3]
                        cnts['st'] += 1
                        for kc in range(8):
                            self.op('pe', lambda: nc.tensor.matmul(p_t[:, :], hT[:, kc, tt * 128:(tt + 1) * 128], w_t[:, kc, :], start=(kc == 0), stop=(kc == 7)), R=[w_t, hT], W=[p_t])
                        if tt % 2 == 0:
                            self.op('act', lambda: nc.scalar.copy(s_t[:, 0, :], p_t[:, :]), R=[p_t], W=[s_t])
                        else:
                            self.op('dve', lambda: nc.vector.tensor_copy(s_t[:, 0, :], p_t[:, :]), R=[p_t], W=[s_t])
                        self.dma('sp', dst[tt * 128:(tt + 1) * 128, :], s_t[:, 0, :], R=[s_t], owner=s_t)
                        yield

        wr = [self.tile("wr", [128, 8, 256], BF16) for _ in range(2)]
        bd = [self.tile("bd", [128, 2, 128], BF16) for _ in range(2)]
        rxs = [self.tile("rxs", [128, 515], F32) for _ in range(2)]
        xr = [self.tile("xr", [128, 512], F32) for _ in range(2)]
        xrb = [self.tile("xrb", [128, 512], BF16) for _ in range(2)]
        rr = [self.tile("rr", [128, 512], F32) for _ in range(2)]
        ii = [self.tile("ii", [128, 512], F32) for _ in range(2)]
        aa = [self.tile("aa", [128, 512], F32) for _ in range(2)]
        a2 = [self.tile("a2", [128, 512], F32) for _ in range(2)]
        bb = [self.tile("bb", [128, 512], F32) for _ in range(2)]
        hs = [self.tile("hs", [128, 512], F32) for _ in range(2)]
        gl = [self.tile("gl", [128, 512], F32) for _ in range(2)]
        yb = [self.tile("yb", [128, 512], BF16) for _ in range(3)]
        pa = self.psum("p1pa")
        px = self.psum("p1px")
        prx, prg = self.npt[0], self.npt[1]

        def load_r(j):
            w_t = wr[j % 2]
            b_t = bd[j % 2]
            self.dma('pool', w_t[:, :, 0:128], wv[:, :, O_RX + j * 128:O_RX + (j + 1) * 128], W=[w_t], owner=w_t)
            self.dma('pool', w_t[:, :, 128:256], wv[:, :, O_RG + j * 128:O_RG + (j + 1) * 128], W=[w_t], owner=w_t)
            self.dma('pool', b_t[:, 0, :], self.bd_d[l, 0, j], W=[b_t], owner=b_t)
            self.dma('pool', b_t[:, 1, :], self.bd_d[l, 1, j], W=[b_t], owner=b_t)

        def rg():
            it = 0
            load_r(0)
            for j in range(8):
                w_t = wr[j % 2]
                b_t = bd[j % 2]
                cw = lambda i: self.pvec[:, pb + PV_RCW + i * 8 + j:pb + PV_RCW + i * 8 + j + 1]
                cb = self.pvec[:, pb + PV_RCB + j:pb + PV_RCB + j + 1]
                ba = self.pvec[:, pb + PV_RBA + j:pb + PV_RBA + j + 1]
                bx = self.pvec[:, pb + PV_RBX + j:pb + PV_RBX + j + 1]
                sa = self.sA[:, l * 8 + j:l * 8 + j + 1]
                sa2 = self.sA2[:, l * 8 + j:l * 8 + j + 1]
                for g in range(NG):
                    k = it % 2
                    it += 1
                    for kc in range(8):
                        self.op('pe', lambda: nc.tensor.matmul(prx[:, :], w_t[:, kc, 0:128], hT[:, kc, g * 512:(g + 1) * 512], start=(kc == 0), stop=(kc == 7)), R=[w_t, hT], W=[prx])
                    for kc in range(8):
                        self.op('pe', lambda: nc.tensor.matmul(prg[:, :], w_t[:, kc, 128:256], hT[:, kc, g * 512:(g + 1) * 512], start=(kc == 0), stop=(kc == 7)), R=[w_t, hT], W=[prg])
                    if g == NG - 1 and j + 1 < 8:
                        load_r(j + 1)
                    rx_t, xr_t, xrb_t = rxs[k], xr[k], xrb[k]
                    if g == 0:
                        self.op('dve', lambda: nc.vector.memset(rx_t[:, 0:3], 0.0), W=[rx_t])
                    else:
                        prev = rxs[1 - k]
                        self.op('dve', lambda: nc.vector.tensor_copy(rx_t[:, 0:3], prev[:, 512:515]), R=[prev], W=[rx_t])
                    self.op('dve', lambda: nc.vector.tensor_copy(rx_t[:, 3:515], prx[:, :]), R=[prx], W=[rx_t])
                    r_t, i_t, a_t, a2_t, b_t2, h_t, g_t = rr[k], ii[k], aa[k], a2[k], bb[k], hs[k], gl[k]
                    self.op('act', lambda: nc.scalar.activation(out=g_t[:], in_=prg[:, :], func=AF.Gelu_apprx_tanh), R=[prg], W=[g_t])
                    self.op('dve', lambda: nc.vector.tensor_scalar(xr_t[:], rx_t[:, 3:515], cw(3), cb, ALU.mult, ALU.add), R=[rx_t, self.pvec], W=[xr_t])
                    for i in range(3):
                        self.op('dve', lambda: nc.vector.scalar_tensor_tensor(xr_t[:], rx_t[:, i:i + 512], cw(i), xr_t[:], ALU.mult, ALU.add), R=[rx_t, self.pvec, xr_t], W=[xr_t])
                    self.op('pool', lambda: nc.gpsimd.tensor_copy(xrb_t[:], xr_t[:]), R=[xr_t], W=[xrb_t])
                    yield
                    self.op('pe', lambda: nc.tensor.matmul(pa[:, :], b_t[:, 0, :], xrb_t[:], start=True, stop=True), R=[b_t, xrb_t], W=[pa])
                    self.op('pe', lambda: nc.tensor.matmul(px[:, :], b_t[:, 1, :], xrb_t[:], start=True, stop=True), R=[b_t, xrb_t], W=[px])
                    self.op('act', lambda: nc.scalar.activation(out=r_t[:], in_=pa[:, :], func=AF.Sigmoid, bias=ba, scale=1.0), R=[pa, self.pvec], W=[r_t])
                    self.op('act', lambda: nc.scalar.activation(out=i_t[:], in_=px[:, :], func=AF.Sigmoid, bias=bx, scale=1.0), R=[px, self.pvec], W=[i_t])
                    self.op('act', lambda: nc.scalar.activation(out=a_t[:], in_=r_t[:], func=AF.Exp, scale=sa), R=[r_t, self.sA], W=[a_t])
                    self.op('act', lambda: nc.scalar.activation(out=a2_t[:], in_=r_t[:], func=AF.Exp, scale=sa2), R=[r_t, self.sA2], W=[a2_t])
                    self.op('act', lambda: nc.scalar.activation(out=a2_t[:], in_=a2_t[:], func=AF.Ln, scale=-1.0, bias=self.one_t[:, 0:1]), R=[a2_t, self.one_t], W=[a2_t])
                    self.op('act', lambda: nc.scalar.activation(out=a2_t[:], in_=a2_t[:], func=AF.Exp, scale=0.5), R=[a2_t], W=[a2_t])
                    self.op('dve', lambda: nc.vector.tensor_tensor(b_t2[:], i_t[:], xr_t[:], ALU.mult), R=[i_t, xr_t], W=[b_t2])
                    self.op('dve', lambda: nc.vector.tensor_tensor(b_t2[:], b_t2[:], a2_t[:], ALU.mult), R=[b_t2, a2_t], W=[b_t2])
                    if g == 0:
                        self.op('dve', lambda: nc.vector.tensor_tensor_scan(h_t[:], a_t[:], b_t2[:], 0.0, ALU.mult, ALU.add), R=[a_t, b_t2], W=[h_t])
                    else:
                        hp = hs[1 - k]
                        self.op('dve', lambda: nc.vector.tensor_tensor_scan(h_t[:], a_t[:], b_t2[:], hp[:, 511:512], ALU.mult, ALU.add), R=[a_t, b_t2, hp], W=[h_t])
                    y_t = yb[it % 3]
                    self.op('dve', lambda: nc.vector.tensor_tensor(y_t[:], h_t[:], g_t[:], ALU.mult), R=[h_t, g_t], W=[y_t])
                    self.dma('sp', self.ybT[j * 128:(j + 1) * 128, g * 512:(g + 1) * 512], y_t[:], R=[y_t], owner=y_t)
                    yield

        gu, gr = units(), rg()
        alive_u = alive_r = True
        while alive_u or alive_r:
            for _ in range(3):
                if alive_u:
                    try:
                        next(gu)
                    except StopIteration:
                        alive_u = False
            if alive_r:
                try:
                    next(gr)
                except StopIteration:
                    alive_r = False
        self.end_phase()

    def attn_tiles(self, g, slope):
        res = []
        for kt in range(4 * g + 4):
            j = kt - 4 * g
            dmin = g * 512 - (kt * 128 + 127)
            if j < 0 and slope * dmin >= ALIBI_CUT:
                continue
            res.append((kt, max(j, 0) * 128, j >= 0))
        return res

    def attn_group(self, Kt, Qt, KR, g, slope, pS, pT, emit_pv):
        nc = self.nc
        tiles = self.attn_tiles(g, slope)
        trimask = self.cbf
        LA = 2
        self.new_call()

        def emit_s(i):
            kt, c0, diag = tiles[i]
            p_t = pS[self.n_s % len(pS)]
            self.n_s += 1
            self.op('pe', lambda: nc.tensor.matmul(p_t[:, c0:512], Kt[0:KR, kt * 128:(kt + 1) * 128], Qt[0:KR, g * 512 + c0:(g + 1) * 512], start=True, stop=not diag), R=[Kt, Qt], W=[p_t])
            if diag:
                self.op('pe', lambda: nc.tensor.matmul(p_t[:, c0:c0 + 128], self.ident[:, 0:128], trimask[:, 128:256], start=False, stop=True), R=[self.cbf], W=[p_t])
            return p_t

        pend = [emit_s(i) for i in range(min(LA, len(tiles)))]
        for i in range(len(tiles)):
            kt, c0, diag = tiles[i]
            p_t = pend.pop(0)
            if i + LA < len(tiles):
                pend.append(emit_s(i + LA))
            self.tick()
            e_t = pT[self.n_e % len(pT)]
            self.n_e += 1
            self.op('act', lambda: nc.scalar.activation(out=e_t[:, c0:512], in_=p_t[:, c0:512], func=AF.Exp, scale=0.125), R=[p_t], W=[e_t])
            emit_pv(i, kt, c0, e_t, i == 0, i == len(tiles) - 1)

    def defer(self, ticks, fn):
        self.epi_q.append([ticks, fn, self.call_id])

    def new_call(self):
        self.call_id += 1
        while self.epi_q and self.epi_q[0][2] <= self.call_id - 2:
            self.epi_q.pop(0)[1]()

    def tick(self):
        self.n_tick += 1
        if self.bg_q and self.n_tick % 2 == 0:
            self.bg_q.pop(0)()
        for it in self.epi_q:
            it[0] -= 1
        while self.epi_q and self.epi_q[0][0] <= 0:
            self.epi_q.pop(0)[1]()

    def flush_epi(self):
        while self.epi_q:
            self.epi_q.pop(0)[1]()

    def phase2(self, l):
        nc = self.nc
        self.begin_phase()
        KR = 86
        Vall = self.tile("Vall", [128, NT, 512], BF16)
        self.dma('sp', Vall[:, 0:16, :], self.va.rearrange("(kt p) c -> p kt c", p=128)[:, 0:16, :], W=[Vall], owner=Vall)
        self.dma('sp', Vall[:, 16:32, :], self.va.rearrange("(kt p) c -> p kt c", p=128)[:, 16:32, :], W=[Vall], owner=Vall)
        gtab = self.tile("gtab", [128, 2 * 32 * 16], F32)
        self.dma('sp', gtab[:], self.gtab_d[:, :], W=[gtab], owner=gtab)
        pastneg = gtab[:, 0:512].rearrange("p (q n) -> p q n", n=16)
        Cq = gtab[:, 512:1024].rearrange("p (q n) -> p q n", n=16)
        Qa = [self.tile("Qa", [KR, T], BF16) for _ in range(2)]
        Ka = [self.tile("Ka", [KR, T], BF16) for _ in range(2)]
        Vh = [self.tile("Vh", [128, NT, 65], BF16) for _ in range(2)]
        q32 = self.tile("q32", [64, T], F32)
        kmean = self.tile("kmean", [64, 16], F32)
        gm = [self.tile("gm", [128, 4, 16], F32) for _ in range(2)]
        mx = [self.tile("mx", [128, 4, 8], F32) for _ in range(2)]
        thr = [self.tile("thr", [128, 4], F32) for _ in range(2)]
        sel = [self.tile("sel", [128, 4, 16], F32) for _ in range(2)]
        mb = [self.tile("mb", [128, 4, 80], BF16) for _ in range(2)]
        for m_t in mb:
            self.op('dve', lambda: nc.vector.memset(m_t[:], 0.0), W=[m_t])
        pT = [self.tile("pT", [128, 512], BF16) for _ in range(4)]
        osb = [self.tile("osb", [65, 512], F32) for _ in range(2)]
        rden = [self.tile("rden", [65, 512], F32) for _ in range(2)]
        yst = [self.tile("yst", [64, 512], BF16) for _ in range(3)]
        pS = [self.psum("pS") for _ in range(3)]
        pO = [self.psum("pO") for _ in range(2)]
        pG = self.psum("pG")
        pM = self.psum("pM")
        pB = self.psum("pB")
        self.n_s = 0
        self.n_e = 0
        self.epi_q = []
        self.bg_q = []
        self.n_tick = 0
        self.call_id = 0
        n_o = 0
        def load_head(h):
            Q_t, K_t, V_t = Qa[h % 2], Ka[h % 2], Vh[h % 2]
            self.dma('sp', Q_t[0:64, :], self.qaT[h * 64:(h + 1) * 64, :], W=[Q_t], owner=Q_t)
            self.dma('pool', Q_t[80:86, :], self.qpos_d[:, :], W=[Q_t], owner=Q_t)
            self.dma('sp', K_t[0:64, :], self.kaT[h * 64:(h + 1) * 64, :], W=[K_t], owner=K_t)
            self.dma('pool', K_t[64:80, :], self.koh_d[:, :], W=[K_t], owner=K_t)
            self.dma('pool', K_t[80:86, :], self.kpos_d[h], W=[K_t], owner=K_t)
            self.op('dve', lambda: nc.vector.tensor_copy(V_t[:, :, 0:64], Vall[:, :, h * 64:(h + 1) * 64]), R=[Vall], W=[V_t])
            self.op('dve', lambda: nc.vector.memset(V_t[:, :, 64:65], 1.0), W=[V_t])

        def gating_chunks(h):
            Q_t, K_t = Qa[h % 2], Ka[h % 2]
            chunks = []

            def c0():
                self.op('dve', lambda: nc.vector.tensor_copy(q32[:, :], Q_t[0:64, :]), R=[Q_t], W=[q32])
                self.op('dve', lambda: nc.vector.tensor_reduce(out=kmean[:, :], in_=K_t[0:64, :].rearrange("p (n k) -> p n k", k=256), axis=AX.X, op=ALU.add), R=[K_t], W=[kmean])
            chunks.append(c0)
            p1s, p2s = [], []
            for g in range(NG):
                def p1(g=g):
                    k = g % 2
                    gm_t, mx_t, thr_t, sel_t, mb_t = gm[k], mx[k], thr[k], sel[k], mb[k]
                    for s_ in range(4):
                        qt = 4 * g + s_
                        self.op('pe', lambda: nc.tensor.matmul(pG[:, s_ * 16:(s_ + 1) * 16], q32[0:64, qt * 128:(qt + 1) * 128], kmean[0:64, :], start=True, stop=True), R=[q32, kmean], W=[pG])
                    self.op('dve', lambda: nc.vector.tensor_tensor(gm_t[:], pG[:, 0:64].rearrange("p (s n) -> p s n", n=16), pastneg[:, 4 * g:4 * g + 4, :], ALU.add), R=[pG, gtab], W=[gm_t])
                    for s_ in range(4):
                        self.op('dve', lambda: nc.vector.max(mx_t[:, s_, :], gm_t[:, s_, :]), R=[gm_t], W=[mx_t])
                    self.op('dve', lambda: nc.vector.tensor_scalar_max(thr_t[:, :], mx_t[:, :, 2], -1e29), R=[mx_t], W=[thr_t])
                    self.op('dve', lambda: nc.vector.tensor_tensor(sel_t[:], gm_t[:], thr_t[:, :].unsqueeze(2).broadcast_to([128, 4, 16]), ALU.is_ge), R=[gm_t, thr_t], W=[sel_t])
                    self.op('dve', lambda: nc.vector.scalar_tensor_tensor(mb_t[:, :, 64:80], sel_t[:], BIG, Cq[:, 4 * g:4 * g + 4, :], ALU.mult, ALU.add), R=[sel_t, gtab], W=[mb_t])

                def p2(g=g):
                    mb_t = mb[g % 2]
                    for s_ in range(4):
                        self.op('pe', lambda: nc.tensor.matmul(pM[0:80, s_ * 128:(s_ + 1) * 128], mb_t[:, s_, :], self.ident[:, 0:128], start=True, stop=True), R=[mb_t, self.ident], W=[pM])
                    self.op('dve', lambda: nc.vector.tensor_copy(Q_t[64:80, g * 512:(g + 1) * 512], pM[64:80, :]), R=[pM], W=[Q_t])
                p1s.append(p1)
                p2s.append(p2)
            chunks.append(p1s[0])
            for g in range(1, NG):
                chunks.append(p1s[g])
                chunks.append(p2s[g - 1])
            chunks.append(p2s[NG - 1])
            return chunks

        load_head(0)
        for c in gating_chunks(0):
            c()
        for h in range(8):
            Q_t, K_t, V_t = Qa[h % 2], Ka[h % 2], Vh[h % 2]
            if h + 1 < 8:
                load_head(h + 1)
                self.bg_q = gating_chunks(h + 1)
            steep = len(self.attn_tiles(4, MOBA_SLOPES[h])) <= 10
            for g in range(NG):
                o_t = pO[n_o % 2]
                os_t = osb[n_o % 2]
                rd_t = rden[n_o % 2]
                n_o += 1

                def pv(i, kt, c0, e_t, first, last):
                    self.op('pe', lambda: nc.tensor.matmul(o_t[0:65, c0:512], V_t[:, kt, :], e_t[:, c0:512], start=first, stop=last), R=[V_t, e_t], W=[o_t])

                self.attn_group(K_t, Q_t, KR, g, MOBA_SLOPES[h], pS, pT, pv)
                y_t = yst[n_o % 3]

                def epiA(o_t=o_t, os_t=os_t):
                    self.op('act', lambda: nc.scalar.copy(os_t[0:65, :], o_t[0:65, :]), R=[o_t], W=[os_t])
                    self.op('pe', lambda: nc.tensor.matmul(pB[0:64, :], self.ones_f[64:65, 0:64], os_t[64:65, :], start=True, stop=True), R=[os_t, self.ones_f], W=[pB])

                def epiB(os_t=os_t, rd_t=rd_t, y_t=y_t, h=h, g=g, steep=steep):
                    if steep:
                        self.op('act', lambda: nc.scalar.activation(out=rd_t[0:64, :], in_=pB[0:64, :], func=AF.Ln), R=[pB], W=[rd_t])
                        self.op('act', lambda: nc.scalar.activation(out=rd_t[0:64, :], in_=rd_t[0:64, :], func=AF.Exp, scale=-1.0), R=[rd_t], W=[rd_t])
                    else:
                        self.op('dve', lambda: nc.vector.reciprocal(rd_t[0:64, :], pB[0:64, :]), R=[pB], W=[rd_t])
                    self.op('dve', lambda: nc.vector.tensor_tensor(y_t[:, :], os_t[0:64, :], rd_t[0:64, :], ALU.mult), R=[os_t, rd_t], W=[y_t])
                    self.dma('sp', self.yaT[h * 64:(h + 1) * 64, g * 512:(g + 1) * 512], y_t[:, :], R=[y_t], owner=y_t)

                self.defer(2, epiA)
                self.defer(5, epiB)
            while self.bg_q:
                self.bg_q.pop(0)()
        self.flush_epi()
        self.end_phase()

    def phase3(self, l):
        nc = self.nc
        self.begin_phase()
        KR = 70
        Vc = self.tile("Vc", [128, NT, 512], BF16)
        self.dma('sp', Vc[:, 0:16, :], self.vc.rearrange("(kt p) c -> p kt c", p=128)[:, 0:16, :], W=[Vc], owner=Vc)
        self.dma('sp', Vc[:, 16:32, :], self.vc.rearrange("(kt p) c -> p kt c", p=128)[:, 16:32, :], W=[Vc], owner=Vc)
        Qc = [self.tile("Qc", [KR, T], BF16) for _ in range(3)]
        Kc = [self.tile("Kc", [KR, T], BF16) for _ in range(3)]
        pT = [self.tile("pT", [128, 512], BF16) for _ in range(4)]
        o1 = [self.tile("o1", [128, 512], F32) for _ in range(2)]
        o2 = [self.tile("o2", [128, 512], F32) for _ in range(2)]
        r1 = [self.tile("r1", [128, 512], F32) for _ in range(2)]
        r2 = [self.tile("r2", [128, 512], F32) for _ in range(2)]
        sq = [self.tile("sq", [128, 512], F32) for _ in range(2)]
        rin = [self.tile("rin", [128, 512], F32) for _ in range(2)]
        yst = [self.tile("yst", [128, 512], BF16) for _ in range(3)]
        pS = [self.psum("pS") for _ in range(3)]
        pO = [self.psum("pO") for _ in range(2)]
        pD = [self.psum("pD") for _ in range(2)]
        pX = self.psum("pX")
        self.n_s = 0
        self.n_e = 0
        self.epi_q = []
        self.bg_q = []
        self.n_tick = 0
        self.call_id = 0
        n_o = 0
        n_qk = 0
        n_acc = 0
        for h in range(4):
            steep = len(self.attn_tiles(4, DIFF_SLOPES[h])) <= 10
            QK = []
            for m in range(2):
                Q_t, K_t = Qc[n_qk % 3], Kc[n_qk % 3]
                n_qk += 1
                r0 = h * 128 + m * 64
                self.dma('sp', Q_t[0:64, :], self.qcT[r0:r0 + 64, :], W=[Q_t], owner=Q_t)
                self.dma('pool', Q_t[64:70, :], self.qpos_d[:, :], W=[Q_t], owner=Q_t)
                self.dma('sp', K_t[0:64, :], self.kcT[r0:r0 + 64, :], W=[K_t], owner=K_t)
                self.dma('pool', K_t[64:70, :], self.kpos_d[8 + h], W=[K_t], owner=K_t)
                QK.append((Q_t, K_t))
            for g in range(NG):
                k = n_o % 2
                n_o += 1
                o1_t, o2_t, r1_t, r2_t, sq_t, ri_t = o1[k], o2[k], r1[k], r2[k], sq[k], rin[k]
                y_t = yst[n_o % 3]
                for m in range(2):
                    Q_t, K_t = QK[m]
                    pO_t, pD_t = pO[n_acc % 2], pD[n_acc % 2]
                    n_acc += 1

                    def pv(i, kt, c0, e_t, first, last, pO_t=pO_t, pD_t=pD_t):
                        self.op('pe', lambda: nc.tensor.matmul(pO_t[:, c0:512], Vc[:, kt, h * 128:(h + 1) * 128], e_t[:, c0:512], start=first, stop=last), R=[Vc, e_t], W=[pO_t])
                        self.op('pe', lambda: nc.tensor.matmul(pD_t[:, c0:512], self.ones_bf[:, 0:128], e_t[:, c0:512], start=first, stop=last), R=[self.ones_bf, e_t], W=[pD_t])

                    self.attn_group(K_t, Q_t, KR, g, DIFF_SLOPES[h], pS, pT, pv)

                    def epiA(m=m, pO_t=pO_t, pD_t=pD_t, o1_t=o1_t, o2_t=o2_t, r1_t=r1_t, r2_t=r2_t, sq_t=sq_t, steep=steep):
                        od, rd = (o1_t, r1_t) if m == 0 else (o2_t, r2_t)
                        if steep:
                            self.op('act', lambda: nc.scalar.copy(od[:, :], pO_t[:, :]), R=[pO_t], W=[od])
                            self.op('act', lambda: nc.scalar.activation(out=rd[:, :], in_=pD_t[:, :], func=AF.Ln), R=[pD_t], W=[rd])
                            self.op('act', lambda: nc.scalar.activation(out=rd[:, :], in_=rd[:, :], func=AF.Exp, scale=-1.0), R=[rd], W=[rd])
                        else:
                            self.op('dve', lambda: nc.vector.tensor_copy(od[:, :], pO_t[:, :]), R=[pO_t], W=[od])
                            self.op('dve', lambda: nc.vector.reciprocal(rd[:, :], pD_t[:, :]), R=[pD_t], W=[rd])
                        self.op('dve', lambda: nc.vector.tensor_tensor(od[:], od[:], rd[:], ALU.mult), R=[od, rd], W=[od])
                        if m == 0:
                            return
                        self.op('dve', lambda: nc.vector.scalar_tensor_tensor(o1_t[:], o2_t[:], self.nlam[:, l:l + 1], o1_t[:], ALU.mult, ALU.add), R=[o1_t, o2_t, self.nlam], W=[o1_t])
                        self.op('pool', lambda: nc.gpsimd.tensor_tensor(sq_t[:], o1_t[:], o1_t[:], ALU.mult), R=[o1_t], W=[sq_t])

                    def epiC(o1_t=o1_t, sq_t=sq_t, ri_t=ri_t, y_t=y_t, h=h, g=g):
                        self.op('pe', lambda: nc.tensor.matmul(pX[:, :], self.ones_f[:, :], sq_t[:, :], start=True, stop=True), R=[self.ones_f, sq_t], W=[pX])
                        self.op('act', lambda: nc.scalar.activation(out=ri_t[:], in_=pX[:, :], func=AF.Ln, scale=1.0 / 128, bias=self.eps_t[:, 0:1]), R=[pX, self.eps_t], W=[ri_t])
                        self.op('act', lambda: nc.scalar.activation(out=ri_t[:], in_=ri_t[:], func=AF.Exp, scale=-0.5), R=[ri_t], W=[ri_t])
                        self.op('dve', lambda: nc.vector.scalar_tensor_tensor(y_t[:], o1_t[:], self.gsub[:, l:l + 1], ri_t[:], ALU.mult, ALU.mult), R=[o1_t, self.gsub, ri_t], W=[y_t])
                        self.dma('sp', self.ycT[h * 128:(h + 1) * 128, g * 512:(g + 1) * 512], y_t[:], R=[y_t], owner=y_t)

                    self.defer(2, epiA)
                    if m == 1:
                        self.defer(8, epiC)
        self.flush_epi()
        self.end_phase()

    def phase5(self, l):
        nc = self.nc
        self.begin_phase()
        xsrc = self.x_in if l == 0 else self.xA
        WA = self.tile("WA", [128, 4, D], BF16)
        WB = self.tile("WB", [128, 8, D], BF16)
        WC = self.tile("WC", [128, 4, D], BF16)
        WO = self.tile("WO", [128, 8, D], BF16)
        self.dma('pool', WA[:], self.w_a[l].rearrange("(kc p) n -> p kc n", p=128), W=[WA], owner=WA)
        self.dma('pool', WB[:], self.w_b[l].rearrange("(kc p) n -> p kc n", p=128), W=[WB], owner=WB)
        self.dma('pool', WC[:], self.w_c[l].rearrange("(kc p) n -> p kc n", p=128), W=[WC], owner=WC)
        self.dma('pool', WO[:], self.w_o[l].rearrange("(kc p) n -> p kc n", p=128), W=[WO], owner=WO)
        ya = [self.tile("ya", [128, 4, 512], BF16) for _ in range(2)]
        yb = [self.tile("yb", [128, 8, 512], BF16) for _ in range(2)]
        yc = [self.tile("yc", [128, 4, 512], BF16) for _ in range(2)]
        gt = [self.tile("gt", [128, 24, 512], BF16) for _ in range(2)]
        mg = [self.tile("mg", [128, 8, 512], BF16) for _ in range(2)]
        t1 = [self.tile("t1", [128, 512], F32) for _ in range(2)]
        t2 = [self.tile("t2", [128, 512], F32) for _ in range(2)]
        xp = [self.tile("xp", [128, D], F32) for _ in range(3)]
        xo = [self.tile("xo", [128, D], F32) for _ in range(3)]
        pA = [self.psum("pA") for _ in range(2)]
        pBk = [self.psum("pBk") for _ in range(2)]
        pC = [self.psum("pC") for _ in range(2)]
        pOut = [self.psum("pOut") for _ in range(2)]
        n = 0
        nx = 0
        npo = 0
        for g in range(NG):
            k = g % 2
            ya_t, yb_t, yc_t, gt_t, mg_t = ya[k], yb[k], yc[k], gt[k], mg[k]
            sl = slice(g * 512, (g + 1) * 512)
            self.dma('sp', ya_t[:], self.yaT.rearrange("(c p) t -> p c t", p=128)[:, :, sl], W=[ya_t], owner=ya_t)
            self.dma('sp', yb_t[:], self.ybT.rearrange("(c p) t -> p c t", p=128)[:, :, sl], W=[yb_t], owner=yb_t)
            self.dma('sp', yc_t[:], self.ycT.rearrange("(c p) t -> p c t", p=128)[:, :, sl], W=[yc_t], owner=yc_t)
            self.dma('sp', gt_t[:, 0:12, :], self.gT.rearrange("(c p) t -> p c t", p=128)[:, 0:12, sl], W=[gt_t], owner=gt_t)
            self.dma('sp', gt_t[:, 12:24, :], self.gT.rearrange("(c p) t -> p c t", p=128)[:, 12:24, sl], W=[gt_t], owner=gt_t)
            for oc in range(8):
                kk = n % 2
                n += 1
                a_p, b_p, c_p = pA[kk], pBk[kk], pC[kk]
                cs = slice(oc * 128, (oc + 1) * 128)
                for kc in range(4):
                    self.op('pe', lambda: nc.tensor.matmul(a_p[:, :], WA[:, kc, cs], ya_t[:, kc, :], start=(kc == 0), stop=(kc == 3)), R=[WA, ya_t], W=[a_p])
                for kc in range(8):
                    self.op('pe', lambda: nc.tensor.matmul(b_p[:, :], WB[:, kc, cs], yb_t[:, kc, :], start=(kc == 0), stop=(kc == 7)), R=[WB, yb_t], W=[b_p])
                for kc in range(4):
                    self.op('pe', lambda: nc.tensor.matmul(c_p[:, :], WC[:, kc, cs], yc_t[:, kc, :], start=(kc == 0), stop=(kc == 3)), R=[WC, yc_t], W=[c_p])
                t1_t, t2_t = t1[kk], t2[kk]
                self.op('dve', lambda: nc.vector.tensor_tensor(t1_t[:], a_p[:, :], gt_t[:, oc, :], ALU.mult), R=[a_p, gt_t], W=[t1_t])
                self.op('dve', lambda: nc.vector.tensor_tensor(t2_t[:], b_p[:, :], gt_t[:, 8 + oc, :], ALU.mult), R=[b_p, gt_t], W=[t2_t])
                self.op('pool', lambda: nc.gpsimd.tensor_tensor(t1_t[:], t1_t[:], t2_t[:], ALU.add), R=[t1_t, t2_t], W=[t1_t])
                self.op('dve', lambda: nc.vector.tensor_tensor(t2_t[:], c_p[:, :], gt_t[:, 16 + oc, :], ALU.mult), R=[c_p, gt_t], W=[t2_t])
                self.op('pool', lambda: nc.gpsimd.tensor_tensor(mg_t[:, oc, :], t1_t[:], t2_t[:], ALU.add), R=[t1_t, t2_t], W=[mg_t])
            for s in range(4):
                x_t = xp[nx % 3]
                xo_t = xo[nx % 3]
                nx += 1
                r0 = g * 512 + s * 128
                self.dma('sp', x_t[:], xsrc[r0:r0 + 128, :], W=[x_t], owner=x_t)
                for half in range(2):
                    o_p = pOut[npo % 2]
                    npo += 1
                    hs_ = slice(half * 512, (half + 1) * 512)
                    for kc in range(8):
                        self.op('pe', lambda: nc.tensor.matmul(o_p[:, :], mg_t[:, kc, s * 128:(s + 1) * 128], WO[:, kc, hs_], start=(kc == 0), stop=(kc == 7)), R=[mg_t, WO], W=[o_p])
                    self.op('dve', lambda: nc.vector.tensor_tensor(xo_t[:, hs_], o_p[:, :], x_t[:, hs_], ALU.add), R=[o_p, x_t], W=[xo_t])
                self.dma('sp', self.xB[r0:r0 + 128, :], xo_t[:], R=[xo_t], owner=xo_t)
        self.end_phase()

    def phase6(self, l):
        nc = self.nc
        pb = l * NPV
        self.begin_phase()
        hT = self.tile("h2T", [128, 8, T], BF16)
        self.norm_transpose(self.xB, pb + PV_GFFN, hT)
        wv = self.f_up[l].rearrange("(kc p) n -> p kc n", p=128)
        wu = [self.tile("wu", [128, 8, 256], BF16) for _ in range(3)]
        cg = [self.tile("cg", [128, 512], F32) for _ in range(3)]
        cv = [self.tile("cv", [128, 512], F32) for _ in range(3)]
        s1 = [self.tile("s1", [128, 2, 520], F32) for _ in range(2)]
        s0 = [self.tile("s0", [128, 2, 520], F32) for _ in range(2)]
        ast = [self.tile("ast", [128, 512], BF16) for _ in range(3)]
        ps = [self.psum("p6ps") for _ in range(6)]

        def load_u(j):
            w_t = wu[j % 3]
            self.dma('pool', w_t[:, :, 0:128], wv[:, :, j * 128:(j + 1) * 128], W=[w_t], owner=w_t)
            self.dma('pool', w_t[:, :, 128:256], wv[:, :, DFF + j * 128:DFF + (j + 1) * 128], W=[w_t], owner=w_t)

        def stageA(it, j, g):
            w_t = wu[j % 3]
            cwg = lambda i: self.pvec[:, pb + PV_FCW + i * 44 + j:pb + PV_FCW + i * 44 + j + 1]
            cwv = lambda i: self.pvec[:, pb + PV_FCW + i * 44 + 22 + j:pb + PV_FCW + i * 44 + 22 + j + 1]
            cbg = self.pvec[:, pb + PV_FCB + j:pb + PV_FCB + j + 1]
            cbv = self.pvec[:, pb + PV_FCB + 22 + j:pb + PV_FCB + 22 + j + 1]
            k = it % 2
            p_g, p_v = ps[(2 * it) % 6], ps[(2 * it + 1) % 6]
            for kc in range(8):
                self.op('pe', lambda: nc.tensor.matmul(p_g[:, :], w_t[:, kc, 0:128], hT[:, kc, g * 512:(g + 1) * 512], start=(kc == 0), stop=(kc == 7)), R=[w_t, hT], W=[p_g])
            for kc in range(8):
                self.op('pe', lambda: nc.tensor.matmul(p_v[:, :], w_t[:, kc, 128:256], hT[:, kc, g * 512:(g + 1) * 512], start=(kc == 0), stop=(kc == 7)), R=[w_t, hT], W=[p_v])
            cg_t, cv_t, s1_t, s0_t = cg[it % 3], cv[it % 3], s1[k], s0[k]
            for (cur, prv, n) in ((s1_t, s1[1 - k], 1), (s0_t, s0[1 - k], 2)):
                if g == 0:
                    self.op('dve', lambda: nc.vector.memset(cur[:, :, 4:4 + n], 0.0), W=[cur])
                else:
                    self.op('dve', lambda: nc.vector.tensor_copy(cur[:, :, 4:4 + n], prv[:, :, 516:516 + n]), R=[prv], W=[cur])
            self.op('act', lambda: nc.scalar.activation(out=cg_t[:], in_=p_g[:, :], func=AF.Identity, scale=cwg(2), bias=cbg), R=[p_g, self.pvec], W=[cg_t])
            self.op('act', lambda: nc.scalar.activation(out=s1_t[:, 0, 5:517], in_=p_g[:, :], func=AF.Identity, scale=cwg(1), bias=self.zero_t[:, 0:1]), R=[p_g, self.pvec], W=[s1_t])
            self.op('act', lambda: nc.scalar.activation(out=s0_t[:, 0, 6:518], in_=p_g[:, :], func=AF.Identity, scale=cwg(0), bias=self.zero_t[:, 0:1]), R=[p_g, self.pvec], W=[s0_t])
            self.op('dve', lambda: nc.vector.tensor_scalar(cv_t[:], p_v[:, :], cwv(2), cbv, ALU.mult, ALU.add), R=[p_v, self.pvec], W=[cv_t])
            self.op('dve', lambda: nc.vector.tensor_scalar_mul(s0_t[:, 1, 6:518], p_v[:, :], cwv(0)), R=[p_v, self.pvec], W=[s0_t])
            self.op('act', lambda: nc.scalar.activation(out=s1_t[:, 1, 5:517], in_=p_v[:, :], func=AF.Identity, scale=cwv(1), bias=self.zero_t[:, 0:1]), R=[p_v, self.pvec], W=[s1_t])
            self.op('dve', lambda: nc.vector.tensor_tensor(cg_t[:], cg_t[:], s1_t[:, 0, 4:516], ALU.add), R=[cg_t, s1_t], W=[cg_t])
            self.op('dve', lambda: nc.vector.tensor_tensor(cg_t[:], cg_t[:], s0_t[:, 0, 4:516], ALU.add), R=[cg_t, s0_t], W=[cg_t])
            self.op('pool', lambda: nc.gpsimd.tensor_tensor(cv_t[:], cv_t[:], s0_t[:, 1, 4:516], ALU.add), R=[cv_t, s0_t], W=[cv_t])
            self.op('pool', lambda: nc.gpsimd.tensor_tensor(cv_t[:], cv_t[:], s1_t[:, 1, 4:516], ALU.add), R=[cv_t, s1_t], W=[cv_t])

        def stageB(it, j, g):
            cg_t, cv_t = cg[it % 3], cv[it % 3]
            self.op('act', lambda: nc.scalar.activation(out=cg_t[:], in_=cg_t[:], func=AF.Gelu_apprx_tanh), R=[cg_t], W=[cg_t])
            a_t = ast[it % 3]
            self.op('pool', lambda: nc.gpsimd.tensor_tensor(a_t[:], cg_t[:], cv_t[:], ALU.mult), R=[cg_t, cv_t], W=[a_t])
            self.dma('sp', self.aT[j * 128:(j + 1) * 128, g * 512:(g + 1) * 512], a_t[:], R=[a_t], owner=a_t)

        load_u(0)
        prev = None
        it = 0
        for j in range(NFC):
            if j + 1 < NFC:
                load_u(j + 1)
            for g in range(NG):
                stageA(it, j, g)
                if prev is not None:
                    stageB(*prev)
                prev = (it, j, g)
                it += 1
        stageB(*prev)
        self.end_phase()
        self.begin_phase()
        WD = self.tile("WD", [128, NFC, D], BF16)
        dv = self.f_dn[l].rearrange("(kc p) n -> p kc n", p=128)
        self.dma('pool', WD[:, 0:11, :], dv[:, 0:11, :], W=[WD], owner=WD)
        self.dma('pool', WD[:, 11:22, :], dv[:, 11:22, :], W=[WD], owner=WD)
        at = [self.tile("at", [128, NFC, 512], BF16) for _ in range(2)]
        xp = [self.tile("xp", [128, D], F32) for _ in range(3)]
        xo = [self.tile("xo", [128, D], F32) for _ in range(3)]
        pOut = [self.psum("pOut") for _ in range(4)]
        nx = 0
        npo = 0
        for g in range(NG):
            a_t = at[g % 2]
            sl = slice(g * 512, (g + 1) * 512)
            av = self.aT.rearrange("(c p) t -> p c t", p=128)
            self.dma('sp', a_t[:, 0:11, :], av[:, 0:11, sl], W=[a_t], owner=a_t)
            self.dma('sp', a_t[:, 11:22, :], av[:, 11:22, sl], W=[a_t], owner=a_t)
            for s in range(4):
                x_t = xp[nx % 3]
                xo_t = xo[nx % 3]
                nx += 1
                r0 = g * 512 + s * 128
                self.dma('sp', x_t[:], self.xB[r0:r0 + 128, :], W=[x_t], owner=x_t)
                for half in range(2):
                    o_p = pOut[npo % 4]
                    npo += 1
                    hs_ = slice(half * 512, (half + 1) * 512)
                    for kc in range(NFC):
                        self.op('pe', lambda: nc.tensor.matmul(o_p[:, :], a_t[:, kc, s * 128:(s + 1) * 128], WD[:, kc, hs_], start=(kc == 0), stop=(kc == NFC - 1)), R=[a_t, WD], W=[o_p])
                    self.op('dve', lambda: nc.vector.tensor_tensor(xo_t[:, hs_], o_p[:, :], x_t[:, hs_], ALU.add), R=[o_p, x_t], W=[xo_t])
                self.dma('sp', self.xA[r0:r0 + 128, :], xo_t[:], R=[xo_t], owner=xo_t)
        self.end_phase()

    def final(self, src):
        nc = self.nc
        self.begin_phase()
        gf = self.tile("gf", [128, D], F32)
        self.dma('sp', gf[:], self.gfin_d[:, :], W=[gf], owner=gf)
        xp = [self.tile("fx", [128, D], F32) for _ in range(3)]
        xo = [self.tile("fo", [128, D], F32) for _ in range(3)]
        junk = self.tile("fjunk", [128, D], BF16)
        ss = [self.tile("fss", [128, 1], F32) for _ in range(3)]
        rs = [self.tile("frs", [128, 1], F32) for _ in range(3)]
        for tt in range(NT):
            x_t, o_t, ss_t, rs_t = xp[tt % 3], xo[tt % 3], ss[tt % 3], rs[tt % 3]
            self.dma('sp', x_t[:], src[tt * 128:(tt + 1) * 128, :], W=[x_t], owner=x_t)
            self.op('dve', lambda: nc.vector.memset(ss_t[:], 0.0), W=[ss_t])
            self.op('act', lambda: nc.scalar.activation(out=junk[:], in_=x_t[:], func=AF.Square, accum_out=ss_t[:]), R=[x_t, ss_t], W=[junk, ss_t])
            self.op('act', lambda: nc.scalar.activation(out=rs_t[:], in_=ss_t[:], func=AF.Sqrt, scale=1.0 / D, bias=self.eps_t[:, 0:1]), R=[ss_t, self.eps_t], W=[rs_t])
            self.op('dve', lambda: nc.vector.reciprocal(rs_t[:], rs_t[:]), R=[rs_t], W=[rs_t])
            self.op('dve', lambda: nc.vector.scalar_tensor_tensor(o_t[:], x_t[:], rs_t[:, 0:1], gf[:], ALU.mult, ALU.mult), R=[x_t, rs_t, gf], W=[o_t])
            self.dma('sp', self.out[tt * 128:(tt + 1) * 128, :], o_t[:], R=[o_t], owner=o_t)
        self.end_phase()

    def build(self):
        self.declare()
        self.setup()
        stop = self.stop_after
        done = False
        for l in range(self.NL):
            for name, fn in (("p1", self.phase1), ("p2", self.phase2), ("p3", self.phase3), ("p5", self.phase5), ("p6", self.phase6)):
                fn(l)
                if stop == (l, name):
                    done = True
                    break
            if done:
                break
        if not done:
            self.final(self.xA)
        self.top.close()
        return self.nc


def _host_consts():
    c = {}
    ident = np.eye(128, dtype=np.float32)
    kk = np.arange(128)[:, None]
    qq = np.arange(128)[None, :]
    tri = np.where(qq >= kk, 0.0, -BIG).astype(np.float32)
    c["cbf"] = np.concatenate([ident, tri], axis=1)
    pos = np.arange(T)
    pa, pbb, pc = pos // 256, (pos // 16) % 16, pos % 16
    c["qpos"] = np.stack([pa, pbb, pc, np.ones(T), np.ones(T), np.ones(T)]).astype(np.float32)
    slopes = [2.0 ** (-8.0 * (h + 1) / 8) for h in range(8)] + [2.0 ** (-8.0 * (h + 1) / 4) for h in range(4)]
    kp = np.zeros((12, 6, T), np.float32)
    for i, s in enumerate(slopes):
        kp[i, 0] = -8 * s * 256
        kp[i, 1] = -8 * s * 16
        kp[i, 2] = -8 * s
        kp[i, 3] = 8 * s * 256 * pa
        kp[i, 4] = 8 * s * 16 * pbb
        kp[i, 5] = 8 * s * pc
    c["kpos"] = kp
    c["koh"] = (np.arange(16)[:, None] == (pos // 256)[None, :]).astype(np.float32)
    qt = np.arange(32)
    jb = qt // 2
    n = np.arange(16)
    pastneg = np.where(n[None, :] < jb[:, None], 0.0, -1e30).astype(np.float32)
    Cq = np.where(n[None, :] == jb[:, None], 0.0, -BIG).astype(np.float32)
    gt = np.concatenate([pastneg.reshape(-1), Cq.reshape(-1)])
    c["gtab"] = np.ascontiguousarray(np.broadcast_to(gt[None, :], (128, gt.size))).astype(np.float32)
    return c


def _fm(v, nch):
    return np.asarray(v, np.float32).reshape(nch, 128).T


def _host_layout(inp):
    m = {}
    pv = np.zeros((128, 4, NPV), np.float32)
    bd = np.zeros((4, 2, 8, 128, 128), np.float32)
    lqk = np.zeros((1, 4, 4, 64), np.float32)
    for l in range(4):
        pv[:, l, PV_GMIX:PV_GMIX + 8] = _fm(inp["norm_mix_g"][l], 8)
        pv[:, l, PV_GFFN:PV_GFFN + 8] = _fm(inp["norm_ffn_g"][l], 8)
        pv[:, l, PV_BGATE:PV_BGATE + 24] = _fm(inp["b_gate"][l], 24)
        for i in range(4):
            pv[:, l, PV_RCW + i * 8:PV_RCW + i * 8 + 8] = _fm(inp["rg_conv_w"][l, i], 8)
        pv[:, l, PV_RCB:PV_RCB + 8] = _fm(inp["rg_conv_b"][l], 8)
        pv[:, l, PV_RBA:PV_RBA + 8] = _fm(inp["rg_b_a"][l], 8)
        pv[:, l, PV_RBX:PV_RBX + 8] = _fm(inp["rg_b_x"][l], 8)
        pv[:, l, PV_RLAM:PV_RLAM + 8] = _fm(inp["rg_lambda"][l], 8)
        pv[:, l, PV_SUBG] = np.asarray(inp["diff_subln_g"][l], np.float32)
        for i in range(3):
            pv[:, l, PV_FCW + i * 44:PV_FCW + i * 44 + 44] = _fm(inp["ffn_conv_w"][l, i], 44)
        pv[:, l, PV_FCB:PV_FCB + 44] = _fm(inp["ffn_conv_b"][l], 44)
        for w, key in enumerate(("rg_w_a", "rg_w_x")):
            for j in range(8):
                for q in range(2):
                    bd[l, w, j, q * 64:(q + 1) * 64, q * 64:(q + 1) * 64] = inp[key][l, 2 * j + q]
        for w, key in enumerate(("diff_lq1", "diff_lk1", "diff_lq2", "diff_lk2")):
            lqk[0, l, w] = inp[key][l]
    m["pvec"] = pv.reshape(128, 4 * NPV)
    m["bdiag"] = bd
    m["lqk"] = np.ascontiguousarray(np.broadcast_to(lqk.reshape(1, -1), (128, 1024)))
    m["gfin"] = np.ascontiguousarray(np.broadcast_to(np.asarray(inp["final_norm_g"], np.float32)[None, :], (128, D)))
    for k in ("w_in", "w_branch_a", "w_branch_b", "w_branch_c", "w_out", "ffn_up", "ffn_down"):
        m[k] = np.ascontiguousarray(np.asarray(inp[k], np.float32))
    m.update(_host_consts())
    return m


def kernel(**inputs):
    x = np.asarray(inputs["x"], np.float32)
    shared = _host_layout(inputs)
    kb = KB(NL=4)
    nc = kb.build()
    in_maps = []
    for c in range(8):
        mm = dict(shared)
        mm["x"] = np.ascontiguousarray(x[c % 4])
        in_maps.append(mm)
    res = run_bass_kernel_spmd(nc, in_maps, core_ids=list(range(8)))
    out = np.stack([np.asarray(res.results[b]["out"], np.float32) for b in range(4)], axis=0)
    return out
```

```python
import contextlib
import numpy as np
import ml_dtypes
import concourse.bass as bass
import concourse.mybir as mybir
from concourse.bass_utils import run_bass_kernel_spmd

F32 = mybir.dt.float32
BF16 = mybir.dt.bfloat16
AF = mybir.ActivationFunctionType
ALU = mybir.AluOpType
AX = mybir.AxisListType

T = 4096
D = 1024
NG = 8
NT = 32
DFF = 2816
NFC = 22
EPS = 1e-6
BIG = 32768.0
ALIBI_CUT = 48.0
MOBA_SLOPES = [2.0 ** (-(h + 1)) for h in range(8)]
DIFF_SLOPES = [2.0 ** (-2.0 * (h + 1)) for h in range(4)]
NPV = 281
O_QA, O_KA, O_VA, O_QC, O_KC, O_VC, O_RX, O_RG, O_GT = 0, 512, 1024, 1536, 2048, 2560, 3072, 4096, 5120
PV_GMIX, PV_GFFN, PV_BGATE, PV_RCW, PV_RCB, PV_RBA, PV_RBX, PV_RLAM, PV_SUBG, PV_FCW, PV_FCB = \
    0, 8, 16, 40, 72, 80, 88, 96, 104, 105, 237


class Tile:
    def __init__(self, kb, t, name):
        self.kb = kb
        self.t = t
        self.name = name
        self.w = None
        self.r = []
        self.dsem = None
        self.dcnt = 0
        self.is_psum = False

    def __getitem__(self, idx):
        return self.t[idx]


class KB:
    def __init__(self, NL=4, debug=False, stop_after=None):
        self.NL = NL
        self.debug = debug
        self.stop_after = stop_after
        nc = self.nc = bass.Bass("TRN2", target_bir_lowering=False)
        self.E = {'pe': nc.tensor, 'act': nc.scalar, 'dve': nc.vector, 'pool': nc.gpsimd, 'sp': nc.sync}
        self.cnt = {e: 0 for e in self.E}
        self.seen = {e: {} for e in self.E}
        self.top = contextlib.ExitStack()
        self.sem = {e: self.top.enter_context(nc.semaphore("s_" + e)) for e in self.E}
        self.bar_sem = self.top.enter_context(nc.semaphore("s_bar"))
        self.nbar = 0
        self.dsem_free = [self.top.enter_context(nc.semaphore("s_d%d" % i)) for i in range(64)]
        self.dsem_cnt = {}
        self.phase = None
        self.phase_tiles = []
        self.persist_tiles = []
        self.uid = 0

    def _mk(self, es, lst, name, shape, dt, psum=False):
        self.uid += 1
        nm = "%s_%d" % (name, self.uid)
        if psum:
            t = es.enter_context(self.nc.psum_tensor(nm, shape, dt))
        else:
            t = es.enter_context(self.nc.sbuf_tensor(nm, shape, dt))
        tl = Tile(self, t, nm)
        tl.is_psum = psum
        lst.append(tl)
        return tl

    def ptile(self, name, shape, dt):
        return self._mk(self.top, self.persist_tiles, name, shape, dt)

    def tile(self, name, shape, dt):
        return self._mk(self.phase, self.phase_tiles, name, shape, dt)

    def psum(self, name, shape=None, dt=F32):
        return self._mk(self.phase, self.phase_tiles, name, shape or [128, 512], dt, psum=True)

    def begin_phase(self):
        self.phase = contextlib.ExitStack()
        self.phase_tiles = []

    def end_phase(self):
        self.barrier()
        for tl in self.phase_tiles:
            if tl.dsem is not None:
                self.dsem_free.append(tl.dsem)
                tl.dsem = None
        self.phase.close()
        self.phase = None
        self.phase_tiles = []

    def _wait(self, e, evs):
        eng = self.E[e]
        need = {}
        for (sem, val, owner) in evs:
            if owner is not None and owner.dsem is sem:
                val = max(val, owner.dcnt)
            if val > need.get(sem, (0, None))[0]:
                need[sem] = (val, sem)
        for sem, (val, _) in need.items():
            if self.seen[e].get(sem, 0) >= val:
                continue
            if sem is self.sem[e] and (e == 'pe' or val <= self.cnt[e] - 4):
                continue
            eng.wait_ge(sem, val)
            self.seen[e][sem] = val

    @staticmethod
    def _gather(R, W):
        evs = []
        for r in R:
            if r.w is not None:
                evs.append(r.w)
            if r.is_psum:
                evs.extend(r.r)
        for w in W:
            if w.w is not None:
                evs.append(w.w)
            evs.extend(w.r)
        return evs

    @staticmethod
    def _record(ev, R, W):
        for r in R:
            r.r = [x for x in r.r if x[0] is not ev[0]] + [ev]
        for w in W:
            w.w = ev
            w.r = []

    def op(self, e, fn, R=(), W=()):
        self._wait(e, self._gather(R, W))
        inst = fn()
        self.cnt[e] += 1
        inst.then_inc(self.sem[e], 1)
        ev = (self.sem[e], self.cnt[e], None)
        self._record(ev, R, W)
        return inst

    def dma(self, q, out, in_, R=(), W=(), owner=None):
        self._wait(q, self._gather(R, W))
        if owner.dsem is None:
            owner.dsem = self.dsem_free.pop()
            owner.dcnt = self.dsem_cnt.get(owner.dsem, 0)
        inst = self.E[q].dma_start(out=out, in_=in_)
        owner.dcnt += 16
        self.dsem_cnt[owner.dsem] = owner.dcnt
        inst.then_inc(owner.dsem, 16)
        ev = (owner.dsem, owner.dcnt, owner)
        self._record(ev, R, W)
        return inst

    def barrier(self):
        sp = self.E['sp']
        for e in self.E:
            if e == 'sp' or self.cnt[e] == 0:
                continue
            if self.seen['sp'].get(self.sem[e], 0) < self.cnt[e]:
                sp.wait_ge(self.sem[e], self.cnt[e])
                self.seen['sp'][self.sem[e]] = self.cnt[e]
        for sem, c in self.dsem_cnt.items():
            if self.seen['sp'].get(sem, 0) < c:
                sp.wait_ge(sem, c)
                self.seen['sp'][sem] = c
        self.nbar += 1
        sp.sem_inc(self.bar_sem, 1)
        for e in self.E:
            if e == 'sp':
                continue
            self.E[e].wait_ge(self.bar_sem, self.nbar)
            for e2 in self.E:
                self.seen[e][self.sem[e2]] = self.cnt[e2]
            for sem, c in self.dsem_cnt.items():
                self.seen[e][sem] = c
        for e2 in self.E:
            self.seen['sp'][self.sem[e2]] = self.cnt[e2]
        for tl in self.phase_tiles + self.persist_tiles:
            tl.w = None
            tl.r = []

    def dram(self, name, shape, dt, kind="Internal"):
        if self.debug and kind == "Internal":
            kind = "ExternalOutput"
        return self.nc.dram_tensor(name, shape, dt, kind=kind).ap()

    def declare(self):
        NL = self.NL
        d = self.dram
        ei = "ExternalInput"
        self.x_in = d("x", [T, D], F32, ei)
        self.w_in = d("w_in", [4, D, 8192], F32, ei)
        self.w_a = d("w_branch_a", [4, 512, D], F32, ei)
        self.w_b = d("w_branch_b", [4, D, D], F32, ei)
        self.w_c = d("w_branch_c", [4, 512, D], F32, ei)
        self.w_o = d("w_out", [4, D, D], F32, ei)
        self.f_up = d("ffn_up", [4, D, 2 * DFF], F32, ei)
        self.f_dn = d("ffn_down", [4, DFF, D], F32, ei)
        self.pvec_d = d("pvec", [128, 4 * NPV], F32, ei)
        self.bd_d = d("bdiag", [4, 2, 8, 128, 128], F32, ei)
        self.lqk_d = d("lqk", [128, 4 * 4 * 64], F32, ei)
        self.gfin_d = d("gfin", [128, D], F32, ei)
        self.cbf_d = d("cbf", [128, 256], F32, ei)
        self.qpos_d = d("qpos", [6, T], F32, ei)
        self.kpos_d = d("kpos", [12, 6, T], F32, ei)
        self.koh_d = d("koh", [16, T], F32, ei)
        self.gtab_d = d("gtab", [128, 2 * 32 * 16], F32, ei)
        self.out = d("out", [T, D], F32, "ExternalOutput")
        self.xA = d("xA", [T, D], F32)
        self.xB = d("xB", [T, D], F32)
        self.qaT = d("qaT", [512, T], BF16)
        self.kaT = d("kaT", [512, T], BF16)
        self.va = d("va", [T, 512], BF16)
        self.qcT = d("qcT", [512, T], BF16)
        self.kcT = d("kcT", [512, T], BF16)
        self.vc = d("vc", [T, 512], BF16)
        self.gT = d("gT", [3072, T], BF16)
        self.yaT = d("yaT", [512, T], BF16)
        self.ybT = d("ybT", [D, T], BF16)
        self.ycT = d("ycT", [512, T], BF16)
        self.aT = d("aT", [DFF, T], BF16)

    def setup(self):
        nc = self.nc
        NL = self.NL
        self.pvec = self.ptile("pvec", [128, 4 * NPV], F32)
        self.cbf = self.ptile("cbf", [128, 256], BF16)
        self.ones_bf = self.ptile("ones_bf", [128, 128], BF16)
        self.ones_f = self.ptile("ones_f", [128, 128], F32)
        self.eps_t = self.ptile("eps", [128, 1], F32)
        self.one_t = self.ptile("one", [128, 1], F32)
        self.zero_t = self.ptile("zero", [128, 1], F32)
        self.sA = self.ptile("sA", [128, 4 * 8], F32)
        self.sA2 = self.ptile("sA2", [128, 4 * 8], F32)
        self.gsub = self.ptile("gsub", [128, 4], F32)
        self.nlam = self.ptile("nlam", [128, 4], F32)
        self.begin_phase()
        self.dma('sp', self.pvec[:], self.pvec_d[:, :], W=[self.pvec], owner=self.pvec)
        self.dma('pool', self.cbf[:], self.cbf_d[:, :], W=[self.cbf], owner=self.cbf)
        self.ident = self.cbf
        self.op('dve', lambda: nc.vector.memset(self.ones_bf[:], 1.0), W=[self.ones_bf])
        self.op('dve', lambda: nc.vector.memset(self.ones_f[:], 1.0), W=[self.ones_f])
        self.op('dve', lambda: nc.vector.memset(self.eps_t[:], EPS), W=[self.eps_t])
        self.op('dve', lambda: nc.vector.memset(self.one_t[:], 1.0), W=[self.one_t])
        self.op('dve', lambda: nc.vector.memset(self.zero_t[:], 0.0), W=[self.zero_t])
        tmp = self.tile("tmp_sa", [128, 4 * 8], F32)
        pv4 = self.pvec[:, :].rearrange("p (l c) -> p l c", l=4)
        lam_v = pv4[:, :, PV_RLAM:PV_RLAM + 8]
        tmp_v = tmp[:, :].rearrange("p (l c) -> p l c", l=4)
        self.op('act', lambda: nc.scalar.activation(out=tmp_v, in_=lam_v, func=AF.Exp, scale=-1.0), R=[self.pvec], W=[tmp])
        self.op('act', lambda: nc.scalar.activation(out=tmp[:], in_=tmp[:], func=AF.Ln, bias=self.one_t[:, 0:1], scale=1.0), R=[tmp, self.one_t], W=[tmp])
        self.op('dve', lambda: nc.vector.tensor_scalar_mul(self.sA[:], tmp[:], -8.0), R=[tmp], W=[self.sA])
        self.op('dve', lambda: nc.vector.tensor_scalar_mul(self.sA2[:], tmp[:], -16.0), R=[tmp], W=[self.sA2])
        import math
        self.lam_init = [0.8 - 0.6 * math.exp(-0.3 * l) for l in range(4)]
        for l in range(4):
            self.op('dve', lambda: nc.vector.tensor_scalar_mul(self.gsub[:, l:l + 1], self.pvec[:, l * NPV + PV_SUBG:l * NPV + PV_SUBG + 1], 1.0 - self.lam_init[l]), R=[self.pvec], W=[self.gsub])
        lqk = self.tile("lqk", [128, 4 * 4 * 64], F32)
        self.dma('sp', lqk[:], self.lqk_d[:, :], W=[lqk], owner=lqk)
        prod = self.tile("lprod", [128, 4 * 2 * 64], F32)
        lv = lqk[:, :].rearrange("p (l w d) -> p l w d", l=4, w=4)
        pvw = prod[:, :].rearrange("p (l w d) -> p l w d", l=4, w=2)
        self.op('dve', lambda: nc.vector.tensor_tensor(pvw[:, :, 0, :], lv[:, :, 0, :], lv[:, :, 1, :], ALU.mult), R=[lqk], W=[prod])
        self.op('dve', lambda: nc.vector.tensor_tensor(pvw[:, :, 1, :], lv[:, :, 2, :], lv[:, :, 3, :], ALU.mult), R=[lqk], W=[prod])
        ssum = self.tile("lsum", [128, 8], F32)
        self.op('dve', lambda: nc.vector.tensor_reduce(out=ssum[:, :], in_=prod[:, :].rearrange("p (a d) -> p a d", d=64), axis=AX.X, op=ALU.add), R=[prod], W=[ssum])
        self.op('act', lambda: nc.scalar.activation(out=ssum[:], in_=ssum[:], func=AF.Exp), R=[ssum], W=[ssum])
        sv = ssum[:, :].rearrange("p (l w) -> p l w", w=2)
        self.op('dve', lambda: nc.vector.tensor_tensor(self.nlam[:, :], sv[:, :, 1], sv[:, :, 0], ALU.subtract), R=[ssum], W=[self.nlam])
        for l in range(4):
            self.op('dve', lambda: nc.vector.tensor_scalar_add(self.nlam[:, l:l + 1], self.nlam[:, l:l + 1], -self.lam_init[l]), R=[self.nlam], W=[self.nlam])
        self.end_phase()

    def norm_transpose(self, xsrc, gcol0, hT):
        nc = self.nc
        xp = [self.tile("nx", [128, D], F32) for _ in range(3)]
        xn = [self.tile("nxn", [128, D], BF16) for _ in range(2)]
        junk = self.tile("njunk", [128, D], BF16)
        ss = [self.tile("nss", [128, 1], F32) for _ in range(3)]
        rs = [self.tile("nrs", [128, 1], F32) for _ in range(3)]
        pt = [self.psum("npt") for _ in range(2)]
        self.npt = pt
        for tt in range(NT):
            x_t = xp[tt % 3]
            xn_t = xn[tt % 2]
            ss_t = ss[tt % 3]
            rs_t = rs[tt % 3]
            self.dma('sp', x_t[:], xsrc[tt * 128:(tt + 1) * 128, :], W=[x_t], owner=x_t)
            self.op('dve', lambda: nc.vector.memset(ss_t[:], 0.0), W=[ss_t])
            self.op('act', lambda: nc.scalar.activation(out=junk[:], in_=x_t[:], func=AF.Square, accum_out=ss_t[:]), R=[x_t, ss_t], W=[junk, ss_t])
            self.op('act', lambda: nc.scalar.activation(out=rs_t[:], in_=ss_t[:], func=AF.Sqrt, scale=1.0 / D, bias=self.eps_t[:, 0:1]), R=[ss_t, self.eps_t], W=[rs_t])
            self.op('dve', lambda: nc.vector.reciprocal(rs_t[:], rs_t[:]), R=[rs_t], W=[rs_t])
            self.op('dve', lambda: nc.vector.tensor_scalar_mul(xn_t[:], x_t[:], rs_t[:, 0:1]), R=[x_t, rs_t], W=[xn_t])
            for half in range(2):
                p_t = pt[half]
                for c4 in range(4):
                    c = half * 4 + c4
                    self.op('pe', lambda: nc.tensor.matmul(p_t[:, c4 * 128:(c4 + 1) * 128], xn_t[:, c * 128:(c + 1) * 128], self.ident[:, 0:128], start=True, stop=True), R=[xn_t, self.ident], W=[p_t])
                gv = self.pvec[:, gcol0 + half * 4:gcol0 + half * 4 + 4].unsqueeze(2).broadcast_to([128, 4, 128])
                self.op('dve', lambda: nc.vector.tensor_tensor(hT[:, half * 4:half * 4 + 4, tt * 128:(tt + 1) * 128], p_t[:, :].rearrange("p (c t) -> p c t", c=4), gv, ALU.mult), R=[p_t, self.pvec], W=[hT])

    def phase1(self, l):
        nc = self.nc
        self.begin_phase()
        xsrc = self.x_in if l == 0 else self.xA
        pb = l * NPV
        hT = self.tile("hT", [128, 8, T], BF16)
        self.norm_transpose(xsrc, pb + PV_GMIX, hT)
        wv = self.w_in[l].rearrange("(kc p) n -> p kc n", p=128)
        wb = [self.tile("wb", [128, 8, 512], BF16) for _ in range(3)]
        st = [self.tile("st", [128, 4, 512], BF16) for _ in range(3)]
        ps = [self.psum("p1ps") for _ in range(4)]
        jobs = [(O_QA, self.qaT, 0, None, 'f'), (O_KA, self.kaT, 0, None, 'f'), (O_QC, self.qcT, 0, None, 'f'), (O_KC, self.kcT, 0, None, 'f')]
        for i in range(6):
            jobs.append((O_GT + i * 512, self.gT, i * 512, pb + PV_BGATE + i * 4, 'f'))
        jobs.append((O_VA, self.va, 0, None, 't'))
        jobs.append((O_VC, self.vc, 0, None, 't'))
        cnts = {'ps': 0, 'st': 0}

        def load_w(ji):
            c0 = jobs[ji][0]
            w_t = wb[ji % 3]
            self.dma('pool', w_t[:], wv[:, :, c0:c0 + 512], W=[w_t], owner=w_t)

        def units():
            load_w(0)
            for ji, (c0, dst, r0, bcol, kind) in enumerate(jobs):
                w_t = wb[ji % 3]
                if ji + 1 < len(jobs):
                    load_w(ji + 1)
                if kind == 'f':
                    dview = dst[r0:r0 + 512, :].rearrange("(m p) t -> p m t", p=128)
                    for g in range(NG):
                        s_t = st[cnts['st'] % 3]
                        cnts['st'] += 1
                        for m in range(4):
                            p_t = ps[cnts['ps'] % 4]
                            cnts['ps'] += 1
                            for kc in range(8):
                                self.op('pe', lambda: nc.tensor.matmul(p_t[:, :], w_t[:, kc, m * 128:(m + 1) * 128], hT[:, kc, g * 512:(g + 1) * 512], start=(kc == 0), stop=(kc == 7)), R=[w_t, hT], W=[p_t])
                            if bcol is not None:
                                self.op('act', lambda: nc.scalar.activation(out=s_t[:, m, :], in_=p_t[:, :], func=AF.Sigmoid, bias=self.pvec[:, bcol + m:bcol + m + 1], scale=1.0), R=[p_t, self.pvec], W=[s_t])
                            elif m % 2 == 0:
                                self.op('act', lambda: nc.scalar.copy(s_t[:, m, :], p_t[:, :]), R=[p_t], W=[s_t])
                            else:
                                self.op('dve', lambda: nc.vector.tensor_copy(s_t[:, m, :], p_t[:, :]), R=[p_t], W=[s_t])
                            if m == 3:
                                self.dma('sp', dview[:, :, g * 512:(g + 1) * 512], s_t[:], R=[s_t], owner=s_t)
                            yield
                else:
                    for tt in range(NT):
                        p_t = ps[cnts['ps'] % 4]
                        cnts['ps'] += 1
                        s_t = st[cnts['st'] % 3]
                        cnts['st'] += 1
                        for kc in range(8):
                            self.op('pe', lambda: nc.tensor.matmul(p_t[:, :], hT[:, kc, tt * 128:(tt + 1) * 128], w_t[:, kc, :], start=(kc == 0), stop=(kc == 7)), R=[w_t, hT], W=[p_t])
                        if tt % 2 == 0:
                            self.op('act', lambda: nc.scalar.copy(s_t[:, 0, :], p_t[:, :]), R=[p_t], W=[s_t])
                        else:
                            self.op('dve', lambda: nc.vector.tensor_copy(s_t[:, 0, :], p_t[:, :]), R=[p_t], W=[s_t])
                        self.dma('sp', dst[tt * 128:(tt + 1) * 128, :], s_t[:, 0, :], R=[s_t], owner=s_t)
                        yield

        wr = [self.tile("wr", [128, 8, 256], BF16) for _ in range(2)]
        bd = [self.tile("bd", [128, 2, 128], BF16) for _ in range(2)]
        rxs = [self.tile("rxs", [128, 515], F32) for _ in range(2)]
        xr = [self.tile("xr", [128, 512], F32) for _ in range(2)]
        xrb = [self.tile("xrb", [128, 512], BF16) for _ in range(2)]
        rr = [self.tile("rr", [128, 512], F32) for _ in range(2)]
        ii = [self.tile("ii", [128, 512], F32) for _ in range(2)]
        aa = [self.tile("aa", [128, 512], F32) for _ in range(2)]
        a2 = [self.tile("a2", [128, 512], F32) for _ in range(2)]
        bb = [self.tile("bb", [128, 512], F32) for _ in range(2)]
        hs = [self.tile("hs", [128, 512], F32) for _ in range(2)]
        gl = [self.tile("gl", [128, 512], F32) for _ in range(2)]
        yb = [self.tile("yb", [128, 512], BF16) for _ in range(3)]
        pa = self.psum("p1pa")
        px = self.psum("p1px")
        prx, prg = self.npt[0], self.npt[1]

        def load_r(j):
            w_t = wr[j % 2]
            b_t = bd[j % 2]
            self.dma('pool', w_t[:, :, 0:128], wv[:, :, O_RX + j * 128:O_RX + (j + 1) * 128], W=[w_t], owner=w_t)
            self.dma('pool', w_t[:, :, 128:256], wv[:, :, O_RG + j * 128:O_RG + (j + 1) * 128], W=[w_t], owner=w_t)
            self.dma('pool', b_t[:, 0, :], self.bd_d[l, 0, j], W=[b_t], owner=b_t)
            self.dma('pool', b_t[:, 1, :], self.bd_d[l, 1, j], W=[b_t], owner=b_t)

        def rg():
            it = 0
            load_r(0)
            for j in range(8):
                w_t = wr[j % 2]
                b_t = bd[j % 2]
                cw = lambda i: self.pvec[:, pb + PV_RCW + i * 8 + j:pb + PV_RCW + i * 8 + j + 1]
                cb = self.pvec[:, pb + PV_RCB + j:pb + PV_RCB + j + 1]
                ba = self.pvec[:, pb + PV_RBA + j:pb + PV_RBA + j + 1]
                bx = self.pvec[:, pb + PV_RBX + j:pb + PV_RBX + j + 1]
                sa = self.sA[:, l * 8 + j:l * 8 + j + 1]
                sa2 = self.sA2[:, l * 8 + j:l * 8 + j + 1]
                for g in range(NG):
                    k = it % 2
                    it += 1
                    for kc in range(8):
                        self.op('pe', lambda: nc.tensor.matmul(prx[:, :], w_t[:, kc, 0:128], hT[:, kc, g * 512:(g + 1) * 512], start=(kc == 0), stop=(kc == 7)), R=[w_t, hT], W=[prx])
                    for kc in range(8):
                        self.op('pe', lambda: nc.tensor.matmul(prg[:, :], w_t[:, kc, 128:256], hT[:, kc, g * 512:(g + 1) * 512], start=(kc == 0), stop=(kc == 7)), R=[w_t, hT], W=[prg])
                    if g == NG - 1 and j + 1 < 8:
                        load_r(j + 1)
                    rx_t, xr_t, xrb_t = rxs[k], xr[k], xrb[k]
                    if g == 0:
                        self.op('dve', lambda: nc.vector.memset(rx_t[:, 0:3], 0.0), W=[rx_t])
                    else:
                        prev = rxs[1 - k]
                        self.op('dve', lambda: nc.vector.tensor_copy(rx_t[:, 0:3], prev[:, 512:515]), R=[prev], W=[rx_t])
                    self.op('dve', lambda: nc.vector.tensor_copy(rx_t[:, 3:515], prx[:, :]), R=[prx], W=[rx_t])
                    r_t, i_t, a_t, a2_t, b_t2, h_t, g_t = rr[k], ii[k], aa[k], a2[k], bb[k], hs[k], gl[k]
                    self.op('act', lambda: nc.scalar.activation(out=g_t[:], in_=prg[:, :], func=AF.Gelu_apprx_tanh), R=[prg], W=[g_t])
                    self.op('dve', lambda: nc.vector.tensor_scalar(xr_t[:], rx_t[:, 3:515], cw(3), cb, ALU.mult, ALU.add), R=[rx_t, self.pvec], W=[xr_t])
                    for i in range(3):
                        self.op('dve', lambda: nc.vector.scalar_tensor_tensor(xr_t[:], rx_t[:, i:i + 512], cw(i), xr_t[:], ALU.mult, ALU.add), R=[rx_t, self.pvec, xr_t], W=[xr_t])
                    self.op('pool', lambda: nc.gpsimd.tensor_copy(xrb_t[:], xr_t[:]), R=[xr_t], W=[xrb_t])
                    yield
                    self.op('pe', lambda: nc.tensor.matmul(pa[:, :], b_t[:, 0, :], xrb_t[:], start=True, stop=True), R=[b_t, xrb_t], W=[pa])
                    self.op('pe', lambda: nc.tensor.matmul(px[:, :], b_t[:, 1, :], xrb_t[:], start=True, stop=True), R=[b_t, xrb_t], W=[px])
                    self.op('act', lambda: nc.scalar.activation(out=r_t[:], in_=pa[:, :], func=AF.Sigmoid, bias=ba, scale=1.0), R=[pa, self.pvec], W=[r_t])
                    self.op('act', lambda: nc.scalar.activation(out=i_t[:], in_=px[:, :], func=AF.Sigmoid, bias=bx, scale=1.0), R=[px, self.pvec], W=[i_t])
                    self.op('act', lambda: nc.scalar.activation(out=a_t[:], in_=r_t[:], func=AF.Exp, scale=sa), R=[r_t, self.sA], W=[a_t])
                    self.op('act', lambda: nc.scalar.activation(out=a2_t[:], in_=r_t[:], func=AF.Exp, scale=sa2), R=[r_t, self.sA2], W=[a2_t])
                    self.op('act', lambda: nc.scalar.activation(out=a2_t[:], in_=a2_t[:], func=AF.Ln, scale=-1.0, bias=self.one_t[:, 0:1]), R=[a2_t, self.one_t], W=[a2_t])
                    self.op('act', lambda: nc.scalar.activation(out=a2_t[:], in_=a2_t[:], func=AF.Exp, scale=0.5), R=[a2_t], W=[a2_t])
                    self.op('dve', lambda: nc.vector.tensor_tensor(b_t2[:], i_t[:], xr_t[:], ALU.mult), R=[i_t, xr_t], W=[b_t2])
                    self.op('dve', lambda: nc.vector.tensor_tensor(b_t2[:], b_t2[:], a2_t[:], ALU.mult), R=[b_t2, a2_t], W=[b_t2])
                    if g == 0:
                        self.op('dve', lambda: nc.vector.tensor_tensor_scan(h_t[:], a_t[:], b_t2[:], 0.0, ALU.mult, ALU.add), R=[a_t, b_t2], W=[h_t])
                    else:
                        hp = hs[1 - k]
                        self.op('dve', lambda: nc.vector.tensor_tensor_scan(h_t[:], a_t[:], b_t2[:], hp[:, 511:512], ALU.mult, ALU.add), R=[a_t, b_t2, hp], W=[h_t])
                    y_t = yb[it % 3]
                    self.op('dve', lambda: nc.vector.tensor_tensor(y_t[:], h_t[:], g_t[:], ALU.mult), R=[h_t, g_t], W=[y_t])
                    self.dma('sp', self.ybT[j * 128:(j + 1) * 128, g * 512:(g + 1) * 512], y_t[:], R=[y_t], owner=y_t)
                    yield

        gu, gr = units(), rg()
        alive_u = alive_r = True
        while alive_u or alive_r:
            for _ in range(3):
                if alive_u:
                    try:
                        next(gu)
                    except StopIteration:
                        alive_u = False
            if alive_r:
                try:
                    next(gr)
                except StopIteration:
                    alive_r = False
        self.end_phase()

    def attn_tiles(self, g, slope):
        res = []
        for kt in range(4 * g + 4):
            j = kt - 4 * g
            dmin = g * 512 - (kt * 128 + 127)
            if j < 0 and slope * dmin >= ALIBI_CUT:
                continue
            res.append((kt, max(j, 0) * 128, j >= 0))
        return res

    def attn_group(self, Kt, Qt, KR, g, slope, pS, pT, emit_pv):
        nc = self.nc
        tiles = self.attn_tiles(g, slope)
        trimask = self.cbf
        LA = 2
        self.new_call()

        def emit_s(i):
            kt, c0, diag = tiles[i]
            p_t = pS[self.n_s % len(pS)]
            self.n_s += 1
            self.op('pe', lambda: nc.tensor.matmul(p_t[:, c0:512], Kt[0:KR, kt * 128:(kt + 1) * 128], Qt[0:KR, g * 512 + c0:(g + 1) * 512], start=True, stop=not diag), R=[Kt, Qt], W=[p_t])
            if diag:
                self.op('pe', lambda: nc.tensor.matmul(p_t[:, c0:c0 + 128], self.ident[:, 0:128], trimask[:, 128:256], start=False, stop=True), R=[self.cbf], W=[p_t])
            return p_t

        pend = [emit_s(i) for i in range(min(LA, len(tiles)))]
        for i in range(len(tiles)):
            kt, c0, diag = tiles[i]
            p_t = pend.pop(0)
            if i + LA < len(tiles):
                pend.append(emit_s(i + LA))
            self.tick()
            e_t = pT[self.n_e % len(pT)]
            self.n_e += 1
            self.op('act', lambda: nc.scalar.activation(out=e_t[:, c0:512], in_=p_t[:, c0:512], func=AF.Exp, scale=0.125), R=[p_t], W=[e_t])
            emit_pv(i, kt, c0, e_t, i == 0, i == len(tiles) - 1)

    def defer(self, ticks, fn):
        self.epi_q.append([ticks, fn, self.call_id])

    def new_call(self):
        self.call_id += 1
        while self.epi_q and self.epi_q[0][2] <= self.call_id - 2:
            self.epi_q.pop(0)[1]()

    def tick(self):
        self.n_tick += 1
        if self.bg_q and self.n_tick % 2 == 0:
            self.bg_q.pop(0)()
        for it in self.epi_q:
            it[0] -= 1
        while self.epi_q and self.epi_q[0][0] <= 0:
            self.epi_q.pop(0)[1]()

    def flush_epi(self):
        while self.epi_q:
            self.epi_q.pop(0)[1]()

    def phase2(self, l):
        nc = self.nc
        self.begin_phase()
        KR = 86
        Vall = self.tile("Vall", [128, NT, 512], BF16)
        self.dma('sp', Vall[:, 0:16, :], self.va.rearrange("(kt p) c -> p kt c", p=128)[:, 0:16, :], W=[Vall], owner=Vall)
        self.dma('sp', Vall[:, 16:32, :], self.va.rearrange("(kt p) c -> p kt c", p=128)[:, 16:32, :], W=[Vall], owner=Vall)
        gtab = self.tile("gtab", [128, 2 * 32 * 16], F32)
        self.dma('sp', gtab[:], self.gtab_d[:, :], W=[gtab], owner=gtab)
        pastneg = gtab[:, 0:512].rearrange("p (q n) -> p q n", n=16)
        Cq = gtab[:, 512:1024].rearrange("p (q n) -> p q n", n=16)
        Qa = [self.tile("Qa", [KR, T], BF16) for _ in range(2)]
        Ka = [self.tile("Ka", [KR, T], BF16) for _ in range(2)]
        Vh = [self.tile("Vh", [128, NT, 65], BF16) for _ in range(2)]
        q32 = self.tile("q32", [64, T], F32)
        kmean = self.tile("kmean", [64, 16], F32)
        gm = [self.tile("gm", [128, 4, 16], F32) for _ in range(2)]
        mx = [self.tile("mx", [128, 4, 8], F32) for _ in range(2)]
        thr = [self.tile("thr", [128, 4], F32) for _ in range(2)]
        sel = [self.tile("sel", [128, 4, 16], F32) for _ in range(2)]
        mb = [self.tile("mb", [128, 4, 80], BF16) for _ in range(2)]
        for m_t in mb:
            self.op('dve', lambda: nc.vector.memset(m_t[:], 0.0), W=[m_t])
        pT = [self.tile("pT", [128, 512], BF16) for _ in range(4)]
        osb = [self.tile("osb", [65, 512], F32) for _ in range(2)]
        rden = [self.tile("rden", [65, 512], F32) for _ in range(2)]
        yst = [self.tile("yst", [64, 512], BF16) for _ in range(3)]
        pS = [self.psum("pS") for _ in range(3)]
        pO = [self.psum("pO") for _ in range(2)]
        pG = self.psum("pG")
        pM = self.psum("pM")
        pB = self.psum("pB")
        self.n_s = 0
        self.n_e = 0
        self.epi_q = []
        self.bg_q = []
        self.n_tick = 0
        self.call_id = 0
        n_o = 0
        def load_head(h):
            Q_t, K_t, V_t = Qa[h % 2], Ka[h % 2], Vh[h % 2]
            self.dma('sp', Q_t[0:64, :], self.qaT[h * 64:(h + 1) * 64, :], W=[Q_t], owner=Q_t)
            self.dma('pool', Q_t[80:86, :], self.qpos_d[:, :], W=[Q_t], owner=Q_t)
            self.dma('sp', K_t[0:64, :], self.kaT[h * 64:(h + 1) * 64, :], W=[K_t], owner=K_t)
            self.dma('pool', K_t[64:80, :], self.koh_d[:, :], W=[K_t], owner=K_t)
            self.dma('pool', K_t[80:86, :], self.kpos_d[h], W=[K_t], owner=K_t)
            self.op('dve', lambda: nc.vector.tensor_copy(V_t[:, :, 0:64], Vall[:, :, h * 64:(h + 1) * 64]), R=[Vall], W=[V_t])
            self.op('dve', lambda: nc.vector.memset(V_t[:, :, 64:65], 1.0), W=[V_t])

        def gating_chunks(h):
            Q_t, K_t = Qa[h % 2], Ka[h % 2]
            chunks = []

            def c0():
                self.op('dve', lambda: nc.vector.tensor_copy(q32[:, :], Q_t[0:64, :]), R=[Q_t], W=[q32])
                self.op('dve', lambda: nc.vector.tensor_reduce(out=kmean[:, :], in_=K_t[0:64, :].rearrange("p (n k) -> p n k", k=256), axis=AX.X, op=ALU.add), R=[K_t], W=[kmean])
            chunks.append(c0)
            p1s, p2s = [], []
            for g in range(NG):
                def p1(g=g):
                    k = g % 2
                    gm_t, mx_t, thr_t, sel_t, mb_t = gm[k], mx[k], thr[k], sel[k], mb[k]
                    for s_ in range(4):
                        qt = 4 * g + s_
                        self.op('pe', lambda: nc.tensor.matmul(pG[:, s_ * 16:(s_ + 1) * 16], q32[0:64, qt * 128:(qt + 1) * 128], kmean[0:64, :], start=True, stop=True), R=[q32, kmean], W=[pG])
                    self.op('dve', lambda: nc.vector.tensor_tensor(gm_t[:], pG[:, 0:64].rearrange("p (s n) -> p s n", n=16), pastneg[:, 4 * g:4 * g + 4, :], ALU.add), R=[pG, gtab], W=[gm_t])
                    for s_ in range(4):
                        self.op('dve', lambda: nc.vector.max(mx_t[:, s_, :], gm_t[:, s_, :]), R=[gm_t], W=[mx_t])
                    self.op('dve', lambda: nc.vector.tensor_scalar_max(thr_t[:, :], mx_t[:, :, 2], -1e29), R=[mx_t], W=[thr_t])
                    self.op('dve', lambda: nc.vector.tensor_tensor(sel_t[:], gm_t[:], thr_t[:, :].unsqueeze(2).broadcast_to([128, 4, 16]), ALU.is_ge), R=[gm_t, thr_t], W=[sel_t])
                    self.op('dve', lambda: nc.vector.scalar_tensor_tensor(mb_t[:, :, 64:80], sel_t[:], BIG, Cq[:, 4 * g:4 * g + 4, :], ALU.mult, ALU.add), R=[sel_t, gtab], W=[mb_t])

                def p2(g=g):
                    mb_t = mb[g % 2]
                    for s_ in range(4):
                        self.op('pe', lambda: nc.tensor.matmul(pM[0:80, s_ * 128:(s_ + 1) * 128], mb_t[:, s_, :], self.ident[:, 0:128], start=True, stop=True), R=[mb_t, self.ident], W=[pM])
                    self.op('dve', lambda: nc.vector.tensor_copy(Q_t[64:80, g * 512:(g + 1) * 512], pM[64:80, :]), R=[pM], W=[Q_t])
                p1s.append(p1)
                p2s.append(p2)
            chunks.append(p1s[0])
            for g in range(1, NG):
                chunks.append(p1s[g])
                chunks.append(p2s[g - 1])
            chunks.append(p2s[NG - 1])
            return chunks

        load_head(0)
        for c in gating_chunks(0):
            c()
        for h in range(8):
            Q_t, K_t, V_t = Qa[h % 2], Ka[h % 2], Vh[h % 2]
            if h + 1 < 8:
                load_head(h + 1)
                self.bg_q = gating_chunks(h + 1)
            steep = len(self.attn_tiles(4, MOBA_SLOPES[h])) <= 10
            for g in range(NG):
                o_t = pO[n_o % 2]
                os_t = osb[n_o % 2]
                rd_t = rden[n_o % 2]
                n_o += 1

                def pv(i, kt, c0, e_t, first, last):
                    self.op('pe', lambda: nc.tensor.matmul(o_t[0:65, c0:512], V_t[:, kt, :], e_t[:, c0:512], start=first, stop=last), R=[V_t, e_t], W=[o_t])

                self.attn_group(K_t, Q_t, KR, g, MOBA_SLOPES[h], pS, pT, pv)
                y_t = yst[n_o % 3]

                def epiA(o_t=o_t, os_t=os_t):
                    self.op('act', lambda: nc.scalar.copy(os_t[0:65, :], o_t[0:65, :]), R=[o_t], W=[os_t])
                    self.op('pe', lambda: nc.tensor.matmul(pB[0:64, :], self.ones_f[64:65, 0:64], os_t[64:65, :], start=True, stop=True), R=[os_t, self.ones_f], W=[pB])

                def epiB(os_t=os_t, rd_t=rd_t, y_t=y_t, h=h, g=g, steep=steep):
                    if steep:
                        self.op('act', lambda: nc.scalar.activation(out=rd_t[0:64, :], in_=pB[0:64, :], func=AF.Ln), R=[pB], W=[rd_t])
                        self.op('act', lambda: nc.scalar.activation(out=rd_t[0:64, :], in_=rd_t[0:64, :], func=AF.Exp, scale=-1.0), R=[rd_t], W=[rd_t])
                    else:
                        self.op('dve', lambda: nc.vector.reciprocal(rd_t[0:64, :], pB[0:64, :]), R=[pB], W=[rd_t])
                    self.op('dve', lambda: nc.vector.tensor_tensor(y_t[:, :], os_t[0:64, :], rd_t[0:64, :], ALU.mult), R=[os_t, rd_t], W=[y_t])
                    self.dma('sp', self.yaT[h * 64:(h + 1) * 64, g * 512:(g + 1) * 512], y_t[:, :], R=[y_t], owner=y_t)

                self.defer(2, epiA)
                self.defer(5, epiB)
            while self.bg_q:
                self.bg_q.pop(0)()
        self.flush_epi()
        self.end_phase()

    def phase3(self, l):
        nc = self.nc
        self.begin_phase()
        KR = 70
        Vc = self.tile("Vc", [128, NT, 512], BF16)
        self.dma('sp', Vc[:, 0:16, :], self.vc.rearrange("(kt p) c -> p kt c", p=128)[:, 0:16, :], W=[Vc], owner=Vc)
        self.dma('sp', Vc[:, 16:32, :], self.vc.rearrange("(kt p) c -> p kt c", p=128)[:, 16:32, :], W=[Vc], owner=Vc)
        Qc = [self.tile("Qc", [KR, T], BF16) for _ in range(3)]
        Kc = [self.tile("Kc", [KR, T], BF16) for _ in range(3)]
        pT = [self.tile("pT", [128, 512], BF16) for _ in range(4)]
        o1 = [self.tile("o1", [128, 512], F32) for _ in range(2)]
        o2 = [self.tile("o2", [128, 512], F32) for _ in range(2)]
        r1 = [self.tile("r1", [128, 512], F32) for _ in range(2)]
        r2 = [self.tile("r2", [128, 512], F32) for _ in range(2)]
        sq = [self.tile("sq", [128, 512], F32) for _ in range(2)]
        rin = [self.tile("rin", [128, 512], F32) for _ in range(2)]
        yst = [self.tile("yst", [128, 512], BF16) for _ in range(3)]
        pS = [self.psum("pS") for _ in range(3)]
        pO = [self.psum("pO") for _ in range(2)]
        pD = [self.psum("pD") for _ in range(2)]
        pX = self.psum("pX")
        self.n_s = 0
        self.n_e = 0
        self.epi_q = []
        self.bg_q = []
        self.n_tick = 0
        self.call_id = 0
        n_o = 0
        n_qk = 0
        n_acc = 0
        for h in range(4):
            steep = len(self.attn_tiles(4, DIFF_SLOPES[h])) <= 10
            QK = []
            for m in range(2):
                Q_t, K_t = Qc[n_qk % 3], Kc[n_qk % 3]
                n_qk += 1
                r0 = h * 128 + m * 64
                self.dma('sp', Q_t[0:64, :], self.qcT[r0:r0 + 64, :], W=[Q_t], owner=Q_t)
                self.dma('pool', Q_t[64:70, :], self.qpos_d[:, :], W=[Q_t], owner=Q_t)
                self.dma('sp', K_t[0:64, :], self.kcT[r0:r0 + 64, :], W=[K_t], owner=K_t)
                self.dma('pool', K_t[64:70, :], self.kpos_d[8 + h], W=[K_t], owner=K_t)
                QK.append((Q_t, K_t))
            for g in range(NG):
                k = n_o % 2
                n_o += 1
                o1_t, o2_t, r1_t, r2_t, sq_t, ri_t = o1[k], o2[k], r1[k], r2[k], sq[k], rin[k]
                y_t = yst[n_o % 3]
                for m in range(2):
                    Q_t, K_t = QK[m]
                    pO_t, pD_t = pO[n_acc % 2], pD[n_acc % 2]
                    n_acc += 1

                    def pv(i, kt, c0, e_t, first, last, pO_t=pO_t, pD_t=pD_t):
                        self.op('pe', lambda: nc.tensor.matmul(pO_t[:, c0:512], Vc[:, kt, h * 128:(h + 1) * 128], e_t[:, c0:512], start=first, stop=last), R=[Vc, e_t], W=[pO_t])
                        self.op('pe', lambda: nc.tensor.matmul(pD_t[:, c0:512], self.ones_bf[:, 0:128], e_t[:, c0:512], start=first, stop=last), R=[self.ones_bf, e_t], W=[pD_t])

                    self.attn_group(K_t, Q_t, KR, g, DIFF_SLOPES[h], pS, pT, pv)

                    def epiA(m=m, pO_t=pO_t, pD_t=pD_t, o1_t=o1_t, o2_t=o2_t, r1_t=r1_t, r2_t=r2_t, sq_t=sq_t, steep=steep):
                        od, rd = (o1_t, r1_t) if m == 0 else (o2_t, r2_t)
                        if steep:
                            self.op('act', lambda: nc.scalar.copy(od[:, :], pO_t[:, :]), R=[pO_t], W=[od])
                            self.op('act', lambda: nc.scalar.activation(out=rd[:, :], in_=pD_t[:, :], func=AF.Ln), R=[pD_t], W=[rd])
                            self.op('act', lambda: nc.scalar.activation(out=rd[:, :], in_=rd[:, :], func=AF.Exp, scale=-1.0), R=[rd], W=[rd])
                        else:
                            self.op('dve', lambda: nc.vector.tensor_copy(od[:, :], pO_t[:, :]), R=[pO_t], W=[od])
                            self.op('dve', lambda: nc.vector.reciprocal(rd[:, :], pD_t[:, :]), R=[pD_t], W=[rd])
                        self.op('dve', lambda: nc.vector.tensor_tensor(od[:], od[:], rd[:], ALU.mult), R=[od, rd], W=[od])
                        if m == 0:
                            return
                        self.op('dve', lambda: nc.vector.scalar_tensor_tensor(o1_t[:], o2_t[:], self.nlam[:, l:l + 1], o1_t[:], ALU.mult, ALU.add), R=[o1_t, o2_t, self.nlam], W=[o1_t])
                        self.op('pool', lambda: nc.gpsimd.tensor_tensor(sq_t[:], o1_t[:], o1_t[:], ALU.mult), R=[o1_t], W=[sq_t])

                    def epiC(o1_t=o1_t, sq_t=sq_t, ri_t=ri_t, y_t=y_t, h=h, g=g):
                        self.op('pe', lambda: nc.tensor.matmul(pX[:, :], self.ones_f[:, :], sq_t[:, :], start=True, stop=True), R=[self.ones_f, sq_t], W=[pX])
                        self.op('act', lambda: nc.scalar.activation(out=ri_t[:], in_=pX[:, :], func=AF.Ln, scale=1.0 / 128, bias=self.eps_t[:, 0:1]), R=[pX, self.eps_t], W=[ri_t])
                        self.op('act', lambda: nc.scalar.activation(out=ri_t[:], in_=ri_t[:], func=AF.Exp, scale=-0.5), R=[ri_t], W=[ri_t])
                        self.op('dve', lambda: nc.vector.scalar_tensor_tensor(y_t[:], o1_t[:], self.gsub[:, l:l + 1], ri_t[:], ALU.mult, ALU.mult), R=[o1_t, self.gsub, ri_t], W=[y_t])
                        self.dma('sp', self.ycT[h * 128:(h + 1) * 128, g * 512:(g + 1) * 512], y_t[:], R=[y_t], owner=y_t)

                    self.defer(2, epiA)
                    if m == 1:
                        self.defer(8, epiC)
        self.flush_epi()
        self.end_phase()

    def phase5(self, l):
        nc = self.nc
        self.begin_phase()
        xsrc = self.x_in if l == 0 else self.xA
        WA = self.tile("WA", [128, 4, D], BF16)
        WB = self.tile("WB", [128, 8, D], BF16)
        WC = self.tile("WC", [128, 4, D], BF16)
        WO = self.tile("WO", [128, 8, D], BF16)
        self.dma('pool', WA[:], self.w_a[l].rearrange("(kc p) n -> p kc n", p=128), W=[WA], owner=WA)
        self.dma('pool', WB[:], self.w_b[l].rearrange("(kc p) n -> p kc n", p=128), W=[WB], owner=WB)
        self.dma('pool', WC[:], self.w_c[l].rearrange("(kc p) n -> p kc n", p=128), W=[WC], owner=WC)
        self.dma('pool', WO[:], self.w_o[l].rearrange("(kc p) n -> p kc n", p=128), W=[WO], owner=WO)
        ya = [self.tile("ya", [128, 4, 512], BF16) for _ in range(2)]
        yb = [self.tile("yb", [128, 8, 512], BF16) for _ in range(2)]
        yc = [self.tile("yc", [128, 4, 512], BF16) for _ in range(2)]
        gt = [self.tile("gt", [128, 24, 512], BF16) for _ in range(2)]
        mg = [self.tile("mg", [128, 8, 512], BF16) for _ in range(2)]
        t1 = [self.tile("t1", [128, 512], F32) for _ in range(2)]
        t2 = [self.tile("t2", [128, 512], F32) for _ in range(2)]
        xp = [self.tile("xp", [128, D], F32) for _ in range(3)]
        xo = [self.tile("xo", [128, D], F32) for _ in range(3)]
        pA = [self.psum("pA") for _ in range(2)]
        pBk = [self.psum("pBk") for _ in range(2)]
        pC = [self.psum("pC") for _ in range(2)]
        pOut = [self.psum("pOut") for _ in range(2)]
        n = 0
        nx = 0
        npo = 0
        for g in range(NG):
            k = g % 2
            ya_t, yb_t, yc_t, gt_t, mg_t = ya[k], yb[k], yc[k], gt[k], mg[k]
            sl = slice(g * 512, (g + 1) * 512)
            self.dma('sp', ya_t[:], self.yaT.rearrange("(c p) t -> p c t", p=128)[:, :, sl], W=[ya_t], owner=ya_t)
            self.dma('sp', yb_t[:], self.ybT.rearrange("(c p) t -> p c t", p=128)[:, :, sl], W=[yb_t], owner=yb_t)
            self.dma('sp', yc_t[:], self.ycT.rearrange("(c p) t -> p c t", p=128)[:, :, sl], W=[yc_t], owner=yc_t)
            self.dma('sp', gt_t[:, 0:12, :], self.gT.rearrange("(c p) t -> p c t", p=128)[:, 0:12, sl], W=[gt_t], owner=gt_t)
            self.dma('sp', gt_t[:, 12:24, :], self.gT.rearrange("(c p) t -> p c t", p=128)[:, 12:24, sl], W=[gt_t], owner=gt_t)
            for oc in range(8):
                kk = n % 2
                n += 1
                a_p, b_p, c_p = pA[kk], pBk[kk], pC[kk]
                cs = slice(oc * 128, (oc + 1) * 128)
                for kc in range(4):
                    self.op('pe', lambda: nc.tensor.matmul(a_p[:, :], WA[:, kc, cs], ya_t[:, kc, :], start=(kc == 0), stop=(kc == 3)), R=[WA, ya_t], W=[a_p])
                for kc in range(8):
                    self.op('pe', lambda: nc.tensor.matmul(b_p[:, :], WB[:, kc, cs], yb_t[:, kc, :], start=(kc == 0), stop=(kc == 7)), R=[WB, yb_t], W=[b_p])
                for kc in range(4):
                    self.op('pe', lambda: nc.tensor.matmul(c_p[:, :], WC[:, kc, cs], yc_t[:, kc, :], start=(kc == 0), stop=(kc == 3)), R=[WC, yc_t], W=[c_p])
                t1_t, t2_t = t1[kk], t2[kk]
                self.op('dve', lambda: nc.vector.tensor_tensor(t1_t[:], a_p[:, :], gt_t[:, oc, :], ALU.mult), R=[a_p, gt_t], W=[t1_t])
                self.op('dve', lambda: nc.vector.tensor_tensor(t2_t[:], b_p[:, :], gt_t[:, 8 + oc, :], ALU.mult), R=[b_p, gt_t], W=[t2_t])
                self.op('pool', lambda: nc.gpsimd.tensor_tensor(t1_t[:], t1_t[:], t2_t[:], ALU.add), R=[t1_t, t2_t], W=[t1_t])
                self.op('dve', lambda: nc.vector.tensor_tensor(t2_t[:], c_p[:, :], gt_t[:, 16 + oc, :], ALU.mult), R=[c_p, gt_t], W=[t2_t])
                self.op('pool', lambda: nc.gpsimd.tensor_tensor(mg_t[:, oc, :], t1_t[:], t2_t[:], ALU.add), R=[t1_t, t2_t], W=[mg_t])
            for s in range(4):
                x_t = xp[nx % 3]
                xo_t = xo[nx % 3]
                nx += 1
                r0 = g * 512 + s * 128
                self.dma('sp', x_t[:], xsrc[r0:r0 + 128, :], W=[x_t], owner=x_t)
                for half in range(2):
                    o_p = pOut[npo % 2]
                    npo += 1
                    hs_ = slice(half * 512, (half + 1) * 512)
                    for kc in range(8):
                        self.op('pe', lambda: nc.tensor.matmul(o_p[:, :], mg_t[:, kc, s * 128:(s + 1) * 128], WO[:, kc, hs_], start=(kc == 0), stop=(kc == 7)), R=[mg_t, WO], W=[o_p])
                    self.op('dve', lambda: nc.vector.tensor_tensor(xo_t[:, hs_], o_p[:, :], x_t[:, hs_], ALU.add), R=[o_p, x_t], W=[xo_t])
                self.dma('sp', self.xB[r0:r0 + 128, :], xo_t[:], R=[xo_t], owner=xo_t)
        self.end_phase()

    def phase6(self, l):
        nc = self.nc
        pb = l * NPV
        self.begin_phase()
        hT = self.tile("h2T", [128, 8, T], BF16)
        self.norm_transpose(self.xB, pb + PV_GFFN, hT)
        wv = self.f_up[l].rearrange("(kc p) n -> p kc n", p=128)
        wu = [self.tile("wu", [128, 8, 256], BF16) for _ in range(3)]
        cg = [self.tile("cg", [128, 512], F32) for _ in range(3)]
        cv = [self.tile("cv", [128, 512], F32) for _ in range(3)]
        s1g = [self.tile("s1g", [128, 520], F32) for _ in range(2)]
        s1v = [self.tile("s1v", [128, 520], F32) for _ in range(2)]
        s0g = [self.tile("s0g", [128, 520], F32) for _ in range(2)]
        s0v = [self.tile("s0v", [128, 520], F32) for _ in range(2)]
        ast = [self.tile("ast", [128, 512], BF16) for _ in range(3)]
        ps = [self.psum("p6ps") for _ in range(6)]

        def load_u(j):
            w_t = wu[j % 3]
            self.dma('pool', w_t[:, :, 0:128], wv[:, :, j * 128:(j + 1) * 128], W=[w_t], owner=w_t)
            self.dma('pool', w_t[:, :, 128:256], wv[:, :, DFF + j * 128:DFF + (j + 1) * 128], W=[w_t], owner=w_t)

        def stageA(it, j, g):
            w_t = wu[j % 3]
            cwg = lambda i: self.pvec[:, pb + PV_FCW + i * 44 + j:pb + PV_FCW + i * 44 + j + 1]
            cwv = lambda i: self.pvec[:, pb + PV_FCW + i * 44 + 22 + j:pb + PV_FCW + i * 44 + 22 + j + 1]
            cbg = self.pvec[:, pb + PV_FCB + j:pb + PV_FCB + j + 1]
            cbv = self.pvec[:, pb + PV_FCB + 22 + j:pb + PV_FCB + 22 + j + 1]
            k = it % 2
            p_g, p_v = ps[(2 * it) % 6], ps[(2 * it + 1) % 6]
            for kc in range(8):
                self.op('pe', lambda: nc.tensor.matmul(p_g[:, :], w_t[:, kc, 0:128], hT[:, kc, g * 512:(g + 1) * 512], start=(kc == 0), stop=(kc == 7)), R=[w_t, hT], W=[p_g])
            for kc in range(8):
                self.op('pe', lambda: nc.tensor.matmul(p_v[:, :], w_t[:, kc, 128:256], hT[:, kc, g * 512:(g + 1) * 512], start=(kc == 0), stop=(kc == 7)), R=[w_t, hT], W=[p_v])
            cg_t, cv_t, s1g_t, s1v_t, s0g_t, s0v_t = cg[it % 3], cv[it % 3], s1g[k], s1v[k], s0g[k], s0v[k]
            for (cur, prv, n) in ((s1g_t, s1g[1 - k], 1), (s1v_t, s1v[1 - k], 1), (s0g_t, s0g[1 - k], 2), (s0v_t, s0v[1 - k], 2)):
                if g == 0:
                    self.op('dve', lambda: nc.vector.memset(cur[:, 4:4 + n], 0.0), W=[cur])
                else:
                    self.op('dve', lambda: nc.vector.tensor_copy(cur[:, 4:4 + n], prv[:, 516:516 + n]), R=[prv], W=[cur])
            self.op('act', lambda: nc.scalar.activation(out=cg_t[:], in_=p_g[:, :], func=AF.Identity, scale=cwg(2), bias=cbg), R=[p_g, self.pvec], W=[cg_t])
            self.op('act', lambda: nc.scalar.activation(out=s1g_t[:, 5:517], in_=p_g[:, :], func=AF.Identity, scale=cwg(1), bias=self.zero_t[:, 0:1]), R=[p_g, self.pvec], W=[s1g_t])
            self.op('act', lambda: nc.scalar.activation(out=s0g_t[:, 6:518], in_=p_g[:, :], func=AF.Identity, scale=cwg(0), bias=self.zero_t[:, 0:1]), R=[p_g, self.pvec], W=[s0g_t])
            self.op('dve', lambda: nc.vector.tensor_scalar(cv_t[:], p_v[:, :], cwv(2), cbv, ALU.mult, ALU.add), R=[p_v, self.pvec], W=[cv_t])
            self.op('dve', lambda: nc.vector.tensor_scalar_mul(s0v_t[:, 6:518], p_v[:, :], cwv(0)), R=[p_v, self.pvec], W=[s0v_t])
            self.op('act', lambda: nc.scalar.activation(out=s1v_t[:, 5:517], in_=p_v[:, :], func=AF.Identity, scale=cwv(1), bias=self.zero_t[:, 0:1]), R=[p_v, self.pvec], W=[s1v_t])
            self.op('dve', lambda: nc.vector.tensor_tensor(cg_t[:], cg_t[:], s1g_t[:, 4:516], ALU.add), R=[cg_t, s1g_t], W=[cg_t])
            self.op('dve', lambda: nc.vector.tensor_tensor(cg_t[:], cg_t[:], s0g_t[:, 4:516], ALU.add), R=[cg_t, s0g_t], W=[cg_t])
            self.op('pool', lambda: nc.gpsimd.tensor_tensor(cv_t[:], cv_t[:], s0v_t[:, 4:516], ALU.add), R=[cv_t, s0v_t], W=[cv_t])
            self.op('pool', lambda: nc.gpsimd.tensor_tensor(cv_t[:], cv_t[:], s1v_t[:, 4:516], ALU.add), R=[cv_t, s1v_t], W=[cv_t])

        def stageB(it, j, g):
            cg_t, cv_t = cg[it % 3], cv[it % 3]
            self.op('act', lambda: nc.scalar.activation(out=cg_t[:], in_=cg_t[:], func=AF.Gelu_apprx_tanh), R=[cg_t], W=[cg_t])
            a_t = ast[it % 3]
            self.op('pool', lambda: nc.gpsimd.tensor_tensor(a_t[:], cg_t[:], cv_t[:], ALU.mult), R=[cg_t, cv_t], W=[a_t])
            self.dma('sp', self.aT[j * 128:(j + 1) * 128, g * 512:(g + 1) * 512], a_t[:], R=[a_t], owner=a_t)

        load_u(0)
        prev = None
        it = 0
        for j in range(NFC):
            if j + 1 < NFC:
                load_u(j + 1)
            for g in range(NG):
                stageA(it, j, g)
                if prev is not None:
                    stageB(*prev)
                prev = (it, j, g)
                it += 1
        stageB(*prev)
        self.end_phase()
        self.begin_phase()
        WD = self.tile("WD", [128, NFC, D], BF16)
        dv = self.f_dn[l].rearrange("(kc p) n -> p kc n", p=128)
        self.dma('pool', WD[:, 0:11, :], dv[:, 0:11, :], W=[WD], owner=WD)
        self.dma('pool', WD[:, 11:22, :], dv[:, 11:22, :], W=[WD], owner=WD)
        at = [self.tile("at", [128, NFC, 512], BF16) for _ in range(2)]
        xp = [self.tile("xp", [128, D], F32) for _ in range(3)]
        xo = [self.tile("xo", [128, D], F32) for _ in range(3)]
        pOut = [self.psum("pOut") for _ in range(4)]
        nx = 0
        npo = 0
        for g in range(NG):
            a_t = at[g % 2]
            sl = slice(g * 512, (g + 1) * 512)
            av = self.aT.rearrange("(c p) t -> p c t", p=128)
            self.dma('sp', a_t[:, 0:11, :], av[:, 0:11, sl], W=[a_t], owner=a_t)
            self.dma('sp', a_t[:, 11:22, :], av[:, 11:22, sl], W=[a_t], owner=a_t)
            for s in range(4):
                x_t = xp[nx % 3]
                xo_t = xo[nx % 3]
                nx += 1
                r0 = g * 512 + s * 128
                self.dma('sp', x_t[:], self.xB[r0:r0 + 128, :], W=[x_t], owner=x_t)
                for half in range(2):
                    o_p = pOut[npo % 4]
                    npo += 1
                    hs_ = slice(half * 512, (half + 1) * 512)
                    for kc in range(NFC):
                        self.op('pe', lambda: nc.tensor.matmul(o_p[:, :], a_t[:, kc, s * 128:(s + 1) * 128], WD[:, kc, hs_], start=(kc == 0), stop=(kc == NFC - 1)), R=[a_t, WD], W=[o_p])
                    self.op('dve', lambda: nc.vector.tensor_tensor(xo_t[:, hs_], o_p[:, :], x_t[:, hs_], ALU.add), R=[o_p, x_t], W=[xo_t])
                self.dma('sp', self.xA[r0:r0 + 128, :], xo_t[:], R=[xo_t], owner=xo_t)
        self.end_phase()

    def final(self, src):
        nc = self.nc
        self.begin_phase()
        gf = self.tile("gf", [128, D], F32)
        self.dma('sp', gf[:], self.gfin_d[:, :], W=[gf], owner=gf)
        xp = [self.tile("fx", [128, D], F32) for _ in range(3)]
        xo = [self.tile("fo", [128, D], F32) for _ in range(3)]
        junk = self.tile("fjunk", [128, D], BF16)
        ss = [self.tile("fss", [128, 1], F32) for _ in range(3)]
        rs = [self.tile("frs", [128, 1], F32) for _ in range(3)]
        for tt in range(NT):
            x_t, o_t, ss_t, rs_t = xp[tt % 3], xo[tt % 3], ss[tt % 3], rs[tt % 3]
            self.dma('sp', x_t[:], src[tt * 128:(tt + 1) * 128, :], W=[x_t], owner=x_t)
            self.op('dve', lambda: nc.vector.memset(ss_t[:], 0.0), W=[ss_t])
            self.op('act', lambda: nc.scalar.activation(out=junk[:], in_=x_t[:], func=AF.Square, accum_out=ss_t[:]), R=[x_t, ss_t], W=[junk, ss_t])
            self.op('act', lambda: nc.scalar.activation(out=rs_t[:], in_=ss_t[:], func=AF.Sqrt, scale=1.0 / D, bias=self.eps_t[:, 0:1]), R=[ss_t, self.eps_t], W=[rs_t])
            self.op('dve', lambda: nc.vector.reciprocal(rs_t[:], rs_t[:]), R=[rs_t], W=[rs_t])
            self.op('dve', lambda: nc.vector.scalar_tensor_tensor(o_t[:], x_t[:], rs_t[:, 0:1], gf[:], ALU.mult, ALU.mult), R=[x_t, rs_t, gf], W=[o_t])
            self.dma('sp', self.out[tt * 128:(tt + 1) * 128, :], o_t[:], R=[o_t], owner=o_t)
        self.end_phase()

    def build(self):
        self.declare()
        self.setup()
        stop = self.stop_after
        done = False
        for l in range(self.NL):
            for name, fn in (("p1", self.phase1), ("p2", self.phase2), ("p3", self.phase3), ("p5", self.phase5), ("p6", self.phase6)):
                fn(l)
                if stop == (l, name):
                    done = True
                    break
            if done:
                break
        if not done:
            self.final(self.xA)
        self.top.close()
        return self.nc


def _host_consts():
    c = {}
    ident = np.eye(128, dtype=np.float32)
    kk = np.arange(128)[:, None]
    qq = np.arange(128)[None, :]
    tri = np.where(qq >= kk, 0.0, -BIG).astype(np.float32)
    c["cbf"] = np.concatenate([ident, tri], axis=1)
    pos = np.arange(T)
    pa, pbb, pc = pos // 256, (pos // 16) % 16, pos % 16
    c["qpos"] = np.stack([pa, pbb, pc, np.ones(T), np.ones(T), np.ones(T)]).astype(np.float32)
    slopes = [2.0 ** (-8.0 * (h + 1) / 8) for h in range(8)] + [2.0 ** (-8.0 * (h + 1) / 4) for h in range(4)]
    kp = np.zeros((12, 6, T), np.float32)
    for i, s in enumerate(slopes):
        kp[i, 0] = -8 * s * 256
        kp[i, 1] = -8 * s * 16
        kp[i, 2] = -8 * s
        kp[i, 3] = 8 * s * 256 * pa
        kp[i, 4] = 8 * s * 16 * pbb
        kp[i, 5] = 8 * s * pc
    c["kpos"] = kp
    c["koh"] = (np.arange(16)[:, None] == (pos // 256)[None, :]).astype(np.float32)
    qt = np.arange(32)
    jb = qt // 2
    n = np.arange(16)
    pastneg = np.where(n[None, :] < jb[:, None], 0.0, -1e30).astype(np.float32)
    Cq = np.where(n[None, :] == jb[:, None], 0.0, -BIG).astype(np.float32)
    gt = np.concatenate([pastneg.reshape(-1), Cq.reshape(-1)])
    c["gtab"] = np.ascontiguousarray(np.broadcast_to(gt[None, :], (128, gt.size))).astype(np.float32)
    return c


def _fm(v, nch):
    return np.asarray(v, np.float32).reshape(nch, 128).T


def _host_layout(inp):
    m = {}
    pv = np.zeros((128, 4, NPV), np.float32)
    bd = np.zeros((4, 2, 8, 128, 128), np.float32)
    lqk = np.zeros((1, 4, 4, 64), np.float32)
    for l in range(4):
        pv[:, l, PV_GMIX:PV_GMIX + 8] = _fm(inp["norm_mix_g"][l], 8)
        pv[:, l, PV_GFFN:PV_GFFN + 8] = _fm(inp["norm_ffn_g"][l], 8)
        pv[:, l, PV_BGATE:PV_BGATE + 24] = _fm(inp["b_gate"][l], 24)
        for i in range(4):
            pv[:, l, PV_RCW + i * 8:PV_RCW + i * 8 + 8] = _fm(inp["rg_conv_w"][l, i], 8)
        pv[:, l, PV_RCB:PV_RCB + 8] = _fm(inp["rg_conv_b"][l], 8)
        pv[:, l, PV_RBA:PV_RBA + 8] = _fm(inp["rg_b_a"][l], 8)
        pv[:, l, PV_RBX:PV_RBX + 8] = _fm(inp["rg_b_x"][l], 8)
        pv[:, l, PV_RLAM:PV_RLAM + 8] = _fm(inp["rg_lambda"][l], 8)
        pv[:, l, PV_SUBG] = np.asarray(inp["diff_subln_g"][l], np.float32)
        for i in range(3):
            pv[:, l, PV_FCW + i * 44:PV_FCW + i * 44 + 44] = _fm(inp["ffn_conv_w"][l, i], 44)
        pv[:, l, PV_FCB:PV_FCB + 44] = _fm(inp["ffn_conv_b"][l], 44)
        for w, key in enumerate(("rg_w_a", "rg_w_x")):
            for j in range(8):
                for q in range(2):
                    bd[l, w, j, q * 64:(q + 1) * 64, q * 64:(q + 1) * 64] = inp[key][l, 2 * j + q]
        for w, key in enumerate(("diff_lq1", "diff_lk1", "diff_lq2", "diff_lk2")):
            lqk[0, l, w] = inp[key][l]
    m["pvec"] = pv.reshape(128, 4 * NPV)
    m["bdiag"] = bd
    m["lqk"] = np.ascontiguousarray(np.broadcast_to(lqk.reshape(1, -1), (128, 1024)))
    m["gfin"] = np.ascontiguousarray(np.broadcast_to(np.asarray(inp["final_norm_g"], np.float32)[None, :], (128, D)))
    for k in ("w_in", "w_branch_a", "w_branch_b", "w_branch_c", "w_out", "ffn_up", "ffn_down"):
        m[k] = np.ascontiguousarray(np.asarray(inp[k], np.float32))
    m.update(_host_consts())
    return m


def kernel(**inputs):
    x = np.asarray(inputs["x"], np.float32)
    shared = _host_layout(inputs)
    kb = KB(NL=4)
    nc = kb.build()
    in_maps = []
    for c in range(8):
        mm = dict(shared)
        mm["x"] = np.ascontiguousarray(x[c % 4])
        in_maps.append(mm)
    res = run_bass_kernel_spmd(nc, in_maps, core_ids=list(range(8)))
    out = np.stack([np.asarray(res.results[b]["out"], np.float32) for b in range(4)], axis=0)
    return out
```
